# Optimizing a Trainium2 kernel written in Bass

```python
import jax, jax.numpy as jnp
from jax import lax
import numpy as np

D_MODEL = 2048
BATCH = 4
SEQ = 2048
DEPTH = 1

D_MIX = D_MODEL
HGRN_HEADS = 8
HGRN_DK = 128
HGRN_DV = 128
HGRN_WIDTH = HGRN_HEADS * HGRN_DV
HGRN_KWIDTH = HGRN_HEADS * HGRN_DK
HGRN_CHUNK = 64
MLA_HEADS = 8
MLA_Q_LORA = 512
MLA_KV_LORA = 256
MLA_NOPE = 128
MLA_ROPE = 64
MLA_V = 128
MLA_QK = MLA_NOPE + MLA_ROPE
MLA_WIDTH = MLA_HEADS * MLA_V
ROPE_BASE = 10000.0
ATTN_BLOCK = 128
N_EXPERTS = 16
EC_CAPACITY = 2
EXPERT_FF = 2048
EPS = 1e-6
IN_SIZES = (HGRN_KWIDTH, HGRN_KWIDTH, HGRN_KWIDTH, HGRN_WIDTH, HGRN_WIDTH, MLA_Q_LORA, MLA_KV_LORA, MLA_ROPE)
D_IN = HGRN_KWIDTH * 3 + HGRN_WIDTH * 2 + MLA_Q_LORA + MLA_KV_LORA + MLA_ROPE

kernel_name = "hymba_hgrn2_mla_expert_choice_encoder_layer"


def _split_points():
    pts, acc = [], 0
    for s in IN_SIZES[:-1]:
        acc += s
        pts.append(acc)
    return pts


def rmsnorm(x, g):
    xf = x.astype(jnp.float32)
    y = xf * lax.rsqrt(jnp.mean(xf * xf, axis=-1, keepdims=True) + EPS)
    return (y * g.astype(jnp.float32)).astype(x.dtype)


def rope_tables(positions):
    inv_freq = 1.0 / (ROPE_BASE ** (jnp.arange(0, MLA_ROPE, 2, dtype=jnp.float32) / MLA_ROPE))
    ang = positions.astype(jnp.float32)[..., None] * inv_freq
    return jnp.cos(ang)[:, :, None, :], jnp.sin(ang)[:, :, None, :]


def apply_rope(x, cos, sin):
    xf = x.astype(jnp.float32)
    half = MLA_ROPE // 2
    x1, x2 = xf[..., :half], xf[..., half:]
    return jnp.concatenate([x1 * cos - x2 * sin, x2 * cos + x1 * sin], axis=-1).astype(x.dtype)


def hgrn2_bidirectional(q, i, zf_fwd, zf_bwd, lb_fwd, lb_bwd):
    B, S, _ = q.shape
    f32 = jnp.float32
    L = HGRN_CHUNK
    N = S // L

    def log_forget(z, lb):
        lb = lb.astype(f32)
        return jnp.log(lb + (1.0 - lb) * jax.nn.sigmoid(z.astype(f32)))

    qf = q.astype(f32)
    vf = i.astype(f32)
    q2 = jnp.concatenate([qf, qf[:, ::-1]], axis=0)
    v2 = jnp.concatenate([vf, vf[:, ::-1]], axis=0)
    g2 = jnp.concatenate([log_forget(zf_fwd, lb_fwd), log_forget(zf_bwd, lb_bwd)[:, ::-1]], axis=0)
    k2 = -jnp.expm1(g2)

    def to_chunks(t, d):
        return t.reshape(2 * B, N, L, HGRN_HEADS, d).transpose(1, 0, 3, 2, 4)

    xs = (to_chunks(q2, HGRN_DK), to_chunks(k2, HGRN_DK), to_chunks(v2, HGRN_DV), to_chunks(g2, HGRN_DK))
    mask = jnp.tril(jnp.ones((L, L), dtype=bool))[:, :, None]

    def step(state, inp):
        qc, kc, vc, gc = inp
        b = jnp.cumsum(gc, axis=2)
        o_inter = jnp.einsum('zhtk,zhkv->zhtv', qc * jnp.exp(b), state)
        diff = b[:, :, :, None, :] - b[:, :, None, :, :]
        decay = jnp.exp(jnp.where(mask, diff, -jnp.inf))
        att = jnp.einsum('zhtsk,zhsk->zhts', decay * qc[:, :, :, None, :], kc)
        o = o_inter + jnp.einsum('zhts,zhsv->zhtv', att, vc)
        b_last = b[:, :, -1:, :]
        state = jnp.exp(b_last[:, :, 0, :])[..., None] * state + \
            jnp.einsum('zhsk,zhsv->zhkv', kc * jnp.exp(b_last - b), vc)
        return state, o

    state0 = jnp.zeros((2 * B, HGRN_HEADS, HGRN_DK, HGRN_DV), f32)
    _, o = lax.scan(step, state0, xs)
    o = o.transpose(1, 0, 3, 2, 4).reshape(2 * B, S, HGRN_HEADS, HGRN_DV)
    return o[:B] + o[B:, ::-1]


def mla_bidirectional(cq, ckv, kpe, positions, qa_norm_g, w_uq, kva_norm_g, w_ukv, q_head_g, k_head_g):
    B, S, _ = cq.shape
    q = (rmsnorm(cq, qa_norm_g) @ w_uq).reshape(B, S, MLA_HEADS, MLA_QK)
    kv = (rmsnorm(ckv, kva_norm_g) @ w_ukv).reshape(B, S, MLA_HEADS, MLA_NOPE + MLA_V)
    k_nope, v = kv[..., :MLA_NOPE], kv[..., MLA_NOPE:]
    k = jnp.concatenate([k_nope, jnp.broadcast_to(kpe[:, :, None, :], (B, S, MLA_HEADS, MLA_ROPE))], axis=-1)
    q = rmsnorm(q, q_head_g)
    k = rmsnorm(k, k_head_g)
    cos, sin = rope_tables(positions)
    q = jnp.concatenate([q[..., :MLA_NOPE], apply_rope(q[..., MLA_NOPE:], cos, sin)], axis=-1)
    k = jnp.concatenate([k[..., :MLA_NOPE], apply_rope(k[..., MLA_NOPE:], cos, sin)], axis=-1)

    nb = S // ATTN_BLOCK
    scale = MLA_QK ** -0.5
    qb = q.reshape(B, nb, ATTN_BLOCK, MLA_HEADS, MLA_QK).transpose(1, 0, 3, 2, 4)
    kt = k.transpose(0, 2, 1, 3)
    vt = v.transpose(0, 2, 1, 3)

    def attend(qblk):
        s = jnp.einsum('bhqd,bhkd->bhqk', qblk, kt).astype(jnp.float32) * scale
        p = jax.nn.softmax(s, axis=-1).astype(vt.dtype)
        return jnp.einsum('bhqk,bhkd->bhqd', p, vt)

    o = lax.map(attend, qb)
    return o.transpose(1, 0, 3, 2, 4).reshape(B, S, MLA_WIDTH)


def expert_choice_ffn(h, w_router, w_gate, w_up, w_down):
    B, S, D = h.shape
    cap = EC_CAPACITY * S // N_EXPERTS
    aff = jax.nn.softmax((h @ w_router).astype(jnp.float32), axis=-1)
    gates, idx = lax.top_k(aff.transpose(0, 2, 1), cap)
    xe = jax.vmap(lambda hb, ib: hb[ib])(h, idx)
    a = jnp.einsum('becd,edf->becf', xe, w_gate)
    u = jnp.einsum('becd,edf->becf', xe, w_up)
    y = jnp.einsum('becf,efd->becd', jax.nn.silu(a) * u, w_down)
    y = y * gates[..., None].astype(y.dtype)
    return jax.vmap(lambda ib, yb: jnp.zeros((S, D), yb.dtype).at[ib.reshape(-1)].add(yb.reshape(-1, D)))(idx, y)


def setup_inputs(seed: int = 0) -> dict:
    key = jax.random.key(seed)
    ks = jax.random.split(key, 24)
    f32 = jnp.float32
    D = D_MODEL

    def nrm(k, shape, fan_in):
        return jax.random.normal(k, shape, f32) * (fan_in ** -0.5)

    def gain(k, shape):
        return 1.0 + 0.02 * jax.random.normal(k, shape, f32)

    x = jax.random.normal(ks[0], (BATCH, SEQ, D), f32)
    c = jax.random.normal(ks[1], (BATCH, D), f32)
    offsets = jax.random.randint(ks[2], (BATCH, 1), 0, 1024, dtype=jnp.int32)
    positions = jnp.arange(SEQ, dtype=jnp.int32)[None, :] + offsets
    return {
        "x": x,
        "c": c,
        "positions": positions,
        "w_ada": 0.5 * nrm(ks[3], (DEPTH, D, 6 * D), D),
        "b_ada": 0.02 * jax.random.normal(ks[4], (DEPTH, 6 * D), f32),
        "norm1_g": gain(ks[5], (DEPTH, D)),
        "w_in": nrm(ks[6], (DEPTH, D, D_IN), D),
        "lb_logits": 0.5 * jax.random.normal(ks[7], (2, DEPTH + 1, HGRN_KWIDTH), f32),
        "hgrn_out_g": gain(ks[8], (DEPTH, HGRN_HEADS, HGRN_DV)),
        "qa_norm_g": gain(ks[9], (DEPTH, MLA_Q_LORA)),
        "w_uq": nrm(ks[10], (DEPTH, MLA_Q_LORA, MLA_HEADS * MLA_QK), MLA_Q_LORA),
        "kva_norm_g": gain(ks[11], (DEPTH, MLA_KV_LORA)),
        "w_ukv": nrm(ks[12], (DEPTH, MLA_KV_LORA, MLA_HEADS * (MLA_NOPE + MLA_V)), MLA_KV_LORA),
        "q_head_g": gain(ks[13], (DEPTH, MLA_QK)),
        "k_head_g": gain(ks[14], (DEPTH, MLA_QK)),
        "w_out": nrm(ks[15], (DEPTH, D_MIX, D), D_MIX),
        "norm2_g": gain(ks[16], (DEPTH, D)),
        "w_router": nrm(ks[17], (DEPTH, D, N_EXPERTS), D),
        "w_gate": nrm(ks[18], (DEPTH, N_EXPERTS, D, EXPERT_FF), D),
        "w_up": nrm(ks[19], (DEPTH, N_EXPERTS, D, EXPERT_FF), D),
        "w_down": nrm(ks[20], (DEPTH, N_EXPERTS, EXPERT_FF, D), EXPERT_FF),
    }


def reference(x, c, positions, w_ada, b_ada, norm1_g, w_in, lb_logits, hgrn_out_g, qa_norm_g, w_uq,
              kva_norm_g, w_ukv, q_head_g, k_head_g, w_out, norm2_g, w_router, w_gate, w_up, w_down):
    B, S, D = x.shape
    lb = jnp.cumsum(jax.nn.softmax(lb_logits.astype(jnp.float32), axis=1), axis=1)
    split_pts = _split_points()
    for l in range(DEPTH):
        mod = (jax.nn.silu(c) @ w_ada[l] + b_ada[l])[:, None, :]
        shift1, scale1, gate1, shift2, scale2, gate2 = jnp.split(mod, 6, axis=-1)

        h = rmsnorm(x, norm1_g[l]) * (1.0 + scale1) + shift1
        proj = h @ w_in[l]
        hq, hf_fwd, hf_bwd, hi, hg, cq, ckv, kpe = jnp.split(proj, split_pts, axis=-1)

        o_hgrn = hgrn2_bidirectional(hq, hi, hf_fwd, hf_bwd, lb[0, l], lb[1, l])
        o_hgrn = rmsnorm(o_hgrn, hgrn_out_g[l]).astype(x.dtype) * \
            jax.nn.silu(hg).reshape(B, S, HGRN_HEADS, HGRN_DV)
        o_hgrn = o_hgrn.reshape(B, S, HGRN_WIDTH)

        o_mla = mla_bidirectional(cq, ckv, kpe, positions, qa_norm_g[l], w_uq[l], kva_norm_g[l],
                                  w_ukv[l], q_head_g[l], k_head_g[l])

        mixed = jnp.concatenate([o_hgrn, o_mla], axis=-1)
        x = x + gate1 * (mixed @ w_out[l])

        h2 = rmsnorm(x, norm2_g[l]) * (1.0 + scale2) + shift2
        x = x + gate2 * expert_choice_ffn(h2, w_router[l], w_gate[l], w_up[l], w_down[l])
    return x
```

```python
import contextlib
import numpy as np
import concourse.bass as bass
import concourse.mybir as mybir
from concourse.bass_utils import run_bass_kernel_spmd

F32 = mybir.dt.float32
BF16 = mybir.dt.bfloat16
I32 = mybir.dt.int32
U32 = mybir.dt.uint32
AF = mybir.ActivationFunctionType
ALU = mybir.AluOpType
AX = mybir.AxisListType

S = 2048
D = 2048
NT = 16
EPS = 1e-6
SB_BASE = 16640
SB_LIMIT = 229376 - 512


class Reg:
    __slots__ = ("name", "w", "rs")

    def __init__(self, name=""):
        self.name = name
        self.w = {}
        self.rs = {}


class T:
    def __init__(self, t, name=""):
        self.t = t
        self.r = Reg(name)

    def __getitem__(self, idx):
        return self.t[idx]


def _regs(xs):
    out = []
    for x in xs:
        if x is None:
            continue
        out.append(x.r if isinstance(x, T) else x)
    return out


class Eng:
    def __init__(self, key, sem):
        self.key = key
        self.sem = sem
        self.count = 0
        self.ops = []
        self.seen = {}


class K:
    def __init__(self, nc, sems):
        self.nc = nc
        self.sems = sems
        self.eng = {k: Eng(k, sems[k]) for k in ("pe", "act", "dve", "pool", "sp")}
        self.dma_pool = {q: [k for k in sems if k.startswith("d" + q)] for q in ("sp", "pool")}
        self.dma_count = {k: 0 for q in self.dma_pool for k in self.dma_pool[q]}
        self.sb_off = SB_BASE
        self.nid = 0

    def sb(self, shape, dtype, name="t"):
        esz = {F32: 4, BF16: 2, I32: 4, U32: 4}[dtype]
        n = 1
        for s in shape[1:]:
            n *= s
        nbytes = (n * esz + 63) // 64 * 64
        off = self.sb_off
        self.sb_off += nbytes
        assert self.sb_off <= SB_LIMIT, f"SBUF overflow at {name}: {self.sb_off}"
        self.nid += 1
        t = self.nc.alloc_sbuf_tensor_at(f"{name}_{self.nid}", list(shape), dtype, offset=off)
        return T(t, name)

    def mark(self):
        return self.sb_off

    def release(self, m):
        self.sb_off = m

    def _need(self, e, toks):
        eng = self.eng[e]
        best = {}
        for t in toks:
            if t is None:
                continue
            sk, v = t
            if best.get(sk, 0) < v:
                best[sk] = v
        for sk, v in best.items():
            if eng.seen.get(sk, 0) >= v:
                continue
            eng.seen[sk] = v
            sem = self.sems[sk]
            eng.ops.append(lambda h, sem=sem, v=v: h.wait_ge(sem, v))

    def _deps(self, e, reads, writes):
        toks = []
        for r in reads:
            for sk, v in r.w.items():
                if sk == e and e == "pe":
                    continue
                toks.append((sk, v))
        for w in writes:
            for sk, v in w.w.items():
                if sk == e and e == "pe":
                    continue
                toks.append((sk, v))
            for sk, v in w.rs.items():
                if sk == e and e == "pe":
                    continue
                toks.append((sk, v))
        self._need(e, toks)

    def _commit(self, tok, reads, writes):
        sk, v = tok
        for r in reads:
            if r.rs.get(sk, 0) < v:
                r.rs[sk] = v
        for w in writes:
            if w.w.get(sk, 0) < v:
                w.w[sk] = v
            w.rs = {}

    def op(self, e, meth, *args, reads=(), writes=(), sig=True, **kw):
        reads = _regs(reads)
        writes = _regs(writes)
        eng = self.eng[e]
        self._deps(e, reads, writes)
        if sig:
            eng.count += 1
            tok = (e, eng.count)
            sem = eng.sem
            eng.ops.append(lambda h: getattr(h, meth)(*args, **kw).then_inc(sem, 1))
        else:
            tok = (e, eng.count + 1)
            eng.ops.append(lambda h: getattr(h, meth)(*args, **kw))
        self._commit(tok, reads, writes)
        return tok

    def dma(self, e, reads=(), writes=(), meth="dma_start", **kw):
        reads = _regs(reads)
        writes = _regs(writes)
        eng = self.eng[e]
        self._deps(e, reads, writes)
        pool = self.dma_pool[e]
        sk = pool.pop(0)
        pool.append(sk)
        if self.dma_count[sk]:
            self._need(e, [(sk, self.dma_count[sk])])
        self.dma_count[sk] += 16
        v = self.dma_count[sk]
        sem = self.sems[sk]
        eng.ops.append(lambda h: getattr(h, meth)(**kw).then_inc(sem, 16))
        tok = (sk, v)
        self._commit(tok, reads, writes)
        return tok

    def barrier(self):
        toks = []
        for kk, eng in self.eng.items():
            if eng.count:
                toks.append((kk, eng.count))
        for sk, v in self.dma_count.items():
            if v:
                toks.append((sk, v))
        for kk in self.eng:
            self._need(kk, toks)

    def replay(self):
        nc = self.nc
        with nc.Block() as block:
            @block.tensor
            def _(h):
                for f in self.eng["pe"].ops:
                    f(h)

            @block.scalar
            def _(h):
                for f in self.eng["act"].ops:
                    f(h)

            @block.vector
            def _(h):
                for f in self.eng["dve"].ops:
                    f(h)

            @block.gpsimd
            def _(h):
                for f in self.eng["pool"].ops:
                    f(h)

            @block.sync
            def _(h):
                for f in self.eng["sp"].ops:
                    f(h)


def build(stop_after=99, debug=False):
    nc = bass.Bass("TRN2", target_bir_lowering=False)

    def din(name, shape, dt=F32):
        return nc.dram_tensor(name, list(shape), dt, kind="ExternalInput").ap()

    x_d = din("x", [S, D])
    c_d = din("c_t", [128, 16])
    pos_d = din("pos_t", [128, 16], I32)
    w_ada_d = din("w_ada", [D, 6 * D])
    b_ada_d = din("b_ada", [1, 6 * D])
    n1g_d = din("norm1_g", [1, D])
    w_in_d = din("w_in", [D, 5952])
    lbl_d = din("lbl", [128, 32])
    hog_d = din("hog", [128, 8])
    qag_d = din("qa_g", [1, 512])
    w_uq_d = din("w_uq", [512, 1536])
    kvag_d = din("kva_g", [1, 256])
    w_ukv_d = din("w_ukv", [256, 2048])
    qhg_d = din("q_hg", [1, 192])
    khg_d = din("k_hg", [1, 192])
    w_out_d = din("w_out", [D, D])
    n2g_d = din("norm2_g", [1, D])
    w_rt_d = din("w_router", [D, 16])
    w_gate_d = din("w_gate", [16, D, D])
    w_up_d = din("w_up", [16, D, D])
    w_down_d = din("w_down", [16, D, D])
    invf_d = din("inv_freq", [1, 32])
    out_d = nc.dram_tensor("out", [S, D], F32, kind="ExternalOutput").ap()
    dbg_kind = "ExternalOutput" if debug else "Internal"
    mod_d = nc.dram_tensor("mod_s", [1, 6 * D], F32, kind=dbg_kind).ap()
    cqkv_d = nc.dram_tensor("cqkv_s", [S, 832], F32, kind=dbg_kind).ap()
    mixT_d = nc.dram_tensor("mixT_s", [NT, 128, 16, 128], BF16, kind=dbg_kind).ap()
    h2_d = nc.dram_tensor("h2_s", [S, D], BF16, kind=dbg_kind).ap()
    aff_d = nc.dram_tensor("aff_s", [16, S], F32, kind=dbg_kind).ap()
    R_mod, R_cqkv, R_mixT, R_h2, R_out = Reg("mod_d"), Reg("cqkv_d"), Reg("mixT_d"), Reg("h2_d"), Reg("out_d")
    R_aff = Reg("aff_d")

    names = ["pe", "act", "dve", "pool", "sp"] + [f"dsp{i}" for i in range(24)] + [f"dpool{i}" for i in range(16)]
    with contextlib.ExitStack() as es:
        sems = {n: nc.alloc_semaphore(n) for n in names}
        k = K(nc, sems)
        PS = [T(nc.alloc_psum_tensor(f"psb{i}", [128, 512], F32), f"ps{i}") for i in range(8)]
        PSB = [p.t[:].bitcast(BF16) for p in PS]

        identb = k.sb([128, 128], BF16, "identb")
        identf = k.sb([128, 128], F32, "identf")
        for idt in (identb, identf):
            k.op("pool", "memset", idt[:], 0.0, writes=[idt])
            k.op("pool", "affine_select", idt[:], idt[:], pattern=[[-1, 128]], compare_op=ALU.not_equal,
                 fill=1.0, base=0, channel_multiplier=1, reads=[idt], writes=[idt])

        def rstd_from_ss(ss_ap, out_ap, tmp_ap, scale, eps, reads, writes_t, tmp_t):
            k.op("dve", "tensor_scalar", tmp_ap, ss_ap, scale, eps, ALU.mult, ALU.add, reads=reads, writes=[tmp_t])
            k.op("act", "activation", tmp_ap, tmp_ap, AF.Sqrt, reads=[tmp_t], writes=[tmp_t])
            k.op("dve", "reciprocal", out_ap, tmp_ap, reads=[tmp_t], writes=[writes_t])

        def transpose_evac(dst_ap, dst_t, src_blocks, src_t, bank, parts_out=128, ncols=128, eng="act"):
            n = len(src_blocks)
            for i, blk in enumerate(src_blocks):
                pin = blk.shape[0]
                k.op("pe", "transpose", PSB[bank][0:parts_out, i * ncols:(i + 1) * ncols], blk, identb[0:pin, 0:pin],
                     reads=[src_t, identb], writes=[PS[bank]], sig=(i == n - 1))
            src = PSB[bank][0:parts_out, 0:n * ncols]
            if len(dst_ap.shape) == 3:
                src = src.rearrange("p (a n) -> p a n", a=n)
            if eng == "act":
                k.op("act", "copy", dst_ap, src, reads=[PS[bank]], writes=[dst_t])
            else:
                k.op("dve", "tensor_copy", dst_ap, src, reads=[PS[bank]], writes=[dst_t])

        m_hT = k.mark()
        hT = k.sb([128, 16, S], BF16, "hT")
        m0 = k.mark()
        ct = k.sb([128, 16], F32, "ct")
        k.dma("sp", out=ct[:], in_=c_d, writes=[ct])
        scb = k.sb([128, 16], BF16, "scb")
        k.op("act", "activation", scb[:], ct[:], AF.Silu, reads=[ct], writes=[scb])
        wada = [k.sb([128, 16, 512], BF16, f"wada{i}") for i in range(2)]
        bblk = [k.sb([1, 512], F32, f"bblk{i}") for i in range(2)]
        gblk = [k.sb([1, 512], F32, f"gblk{i}") for i in range(2)]
        mblk = [k.sb([1, 512], F32, f"mblk{i}") for i in range(2)]

        def mod_block(nb):
            wb, bb, gb, mm_ = wada[nb % 2], bblk[nb % 2], gblk[nb % 2], mblk[nb % 2]
            cs_ = slice(nb * 512, (nb + 1) * 512)
            k.dma("pool", out=wb[:], in_=w_ada_d[:, cs_].rearrange("(kc p) n -> p kc n", p=128), writes=[wb])
            k.dma("sp", out=bb[:], in_=b_ada_d[0:1, cs_], writes=[bb])
            gsrc = None
            if 4 <= nb < 8:
                gsrc = n1g_d[0:1, (nb - 4) * 512:(nb - 3) * 512]
            elif 16 <= nb < 20:
                gsrc = n2g_d[0:1, (nb - 16) * 512:(nb - 15) * 512]
            if gsrc is not None:
                k.dma("sp", out=gb[:], in_=gsrc, writes=[gb])
            pb = PS[6 + nb % 2]
            for kc in range(16):
                k.op("pe", "matmul", pb[0:1, :], lhsT=scb[:, kc:kc + 1], rhs=wb[:, kc, :], start=(kc == 0),
                     stop=(kc == 15), reads=[scb, wb], writes=[pb], sig=(kc == 15))
            k.op("dve", "tensor_tensor", mm_[:], pb[0:1, :], bb[:], ALU.add, reads=[pb, bb], writes=[mm_])
            if gsrc is not None:
                k.op("dve", "scalar_tensor_tensor", mm_[:], mm_[:], 1.0, gb[:], ALU.add, ALU.mult, reads=[mm_, gb],
                     writes=[mm_])
            k.dma("sp", out=mod_d[0:1, cs_], in_=mm_[:], reads=[mm_], writes=[R_mod])

        for nb in range(8):
            mod_block(nb)

        def bcast_load(dst, off, n=D):
            k.dma("sp", out=dst[:], in_=mod_d[0:1, off:off + n].partition_broadcast(128), reads=[R_mod], writes=[dst])

        m1 = k.mark()
        G1b = k.sb([128, D], F32, "G1b")
        Sh1b = k.sb([128, D], F32, "Sh1b")
        bcast_load(G1b, D)
        bcast_load(Sh1b, 0)
        xt = [k.sb([128, D], F32, f"xt{i}") for i in range(2)]
        junk = k.sb([128, D], BF16, "junk")
        tmpf = k.sb([128, D], F32, "tmpf")
        hb = [k.sb([128, D], BF16, f"hb{i}") for i in range(2)]
        st = [k.sb([128, 4], F32, f"st{i}") for i in range(2)]
        for t in range(NT):
            xx, hh, ss = xt[t % 2], hb[t % 2], st[t % 2]
            k.dma("sp", out=xx[:], in_=x_d[t * 128:(t + 1) * 128, :], writes=[xx])
            mod_block(8 + t)
            k.op("act", "activation", junk[:], xx[:], AF.Square, accum_out=ss[:, 0:1], reads=[xx], writes=[junk, ss])
            rstd_from_ss(ss[:, 0:1], ss[:, 2:3], ss[:, 1:2], 1.0 / D, EPS, [ss], ss, ss)
            k.op("dve", "scalar_tensor_tensor", tmpf[:], xx[:], ss[:, 2:3], G1b[:], ALU.mult, ALU.mult,
                 reads=[xx, ss, G1b], writes=[tmpf])
            k.op("dve", "tensor_tensor", hh[:], tmpf[:], Sh1b[:], ALU.add, reads=[tmpf, Sh1b], writes=[hh])
            for half in range(2):
                bank = (t % 2) * 2 + half
                blocks = [hh[:, (half * 8 + i) * 128:(half * 8 + i + 1) * 128] for i in range(8)]
                transpose_evac(hT[:, half * 8:(half + 1) * 8, t * 128:(t + 1) * 128], hT, blocks, hh, bank)
        k.barrier()
        k.release(m0)
        if stop_after <= 1:
            return nc, k

        m2 = k.mark()
        wm = k.sb([128, 16, 832], BF16, "wm")
        k.dma("pool", out=wm[:], in_=w_in_d[:, 5120:5952].rearrange("(kc p) n -> p kc n", p=128), writes=[wm])
        stg = [k.sb([128, 832], F32, f"stg{i}") for i in range(2)]
        for t in range(NT):
            sg_ = stg[t % 2]
            for j, (c0, c1) in enumerate(((0, 512), (512, 832))):
                pb = PS[(t % 2) * 2 + j]
                for kc in range(16):
                    k.op("pe", "matmul", pb[:, 0:c1 - c0], lhsT=hT[:, kc, t * 128:(t + 1) * 128], rhs=wm[:, kc, c0:c1],
                         start=(kc == 0), stop=(kc == 15), reads=[hT, wm], writes=[pb], sig=(kc == 15))
                k.op("act", "copy", sg_[:, c0:c1], pb[:, 0:c1 - c0], reads=[pb], writes=[sg_])
            k.dma("sp", out=cqkv_d[t * 128:(t + 1) * 128, :], in_=sg_[:], reads=[sg_], writes=[R_cqkv])
        k.barrier()
        k.release(m2)
        if stop_after <= 2:
            return nc, k

        m3 = k.mark()
        L = 64
        NCH = S // L
        maskf = k.sb([64, 64], F32, "maskf")
        maskb = k.sb([64, 64], F32, "maskb")
        k.op("pool", "memset", maskf[:], 1.0, writes=[maskf])
        k.op("pool", "affine_select", maskf[:], maskf[:], pattern=[[1, 64]], compare_op=ALU.is_ge, fill=0.0, base=0,
             channel_multiplier=-1, reads=[maskf], writes=[maskf])
        k.op("pool", "memset", maskb[:], 1.0, writes=[maskb])
        k.op("pool", "affine_select", maskb[:], maskb[:], pattern=[[-1, 64]], compare_op=ALU.is_ge, fill=0.0, base=0,
             channel_multiplier=1, reads=[maskb], writes=[maskb])
        masks = (maskf, maskb)
        scanm = k.sb([128, S], BF16, "scanm")
        k.op("pool", "memset", scanm[:], 1.0, writes=[scanm])
        k.op("pool", "memset", scanm[:].rearrange("p (c l) -> p c l", l=L)[:, :, 0:1], 0.0, writes=[scanm])
        lbl = k.sb([128, 32], F32, "lbl")
        k.dma("sp", out=lbl[:], in_=lbl_d, writes=[lbl])
        lbv = k.sb([128, 3, 16], F32, "lbv")
        l4 = lbl[:].rearrange("p (d s h) -> p d s h", d=2, s=2)
        lb_dh = lbv[:, 0, :].rearrange("p (d h) -> p d h", d=2)
        k.op("dve", "tensor_tensor", lb_dh, l4[:, :, 0, :], l4[:, :, 1, :], ALU.subtract, reads=[lbl], writes=[lbv])
        k.op("act", "activation", lbv[:, 0, :], lbv[:, 0, :], AF.Sigmoid, reads=[lbv], writes=[lbv])
        k.op("dve", "tensor_scalar", lbv[:, 1, :], lbv[:, 0, :], -1.0, 1.0, ALU.mult, ALU.add, reads=[lbv], writes=[lbv])
        k.op("dve", "tensor_scalar", lbv[:, 2, :], lbv[:, 0, :], -1.0, None, ALU.add, reads=[lbv], writes=[lbv])
        hog = k.sb([128, 8], F32, "hog")
        k.dma("sp", out=hog[:], in_=hog_d, writes=[hog])

        wh1 = k.sb([128, 16, 5, 128], BF16, "wh")
        qf = k.sb([128, S], F32, "qf")
        scr = k.sb([128, 9216], F32, "scr")
        tA = T(scr.t[:, 0:2048], "tA")
        tB = T(scr.t[:, 2048:4096], "tB")
        tC = T(scr.t[:, 4096:6144], "tC")
        tD = T(scr.t[:, 6144:8192], "tD")
        tE = T(scr.t[:, 8192:9216], "tE")
        Uall = scr.t[:, 0:4096]
        Sall = scr.t[:, 4096:8192]
        decrep = scr.t[:, 8192:9216]
        sgT = k.sb([128, S], BF16, "sgT")
        VT = k.sb([128, S], BF16, "VT")
        khT = [k.sb([128, S], BF16, f"khT{d}") for d in range(2)]
        qtT = [k.sb([128, S], BF16, f"qtT{d}") for d in range(2)]
        dec = [k.sb([128, NCH], F32, f"dec{d}") for d in range(2)]
        decj = [dec[0], k.sb([128, NCH], F32, "decj1")]
        decz = k.sb([128, NCH], F32, "decz")
        Vt = k.sb([64, NCH, 128], BF16, "Vt")
        khTok = k.sb([64, NCH, 128], BF16, "khTok")
        ATs = [k.sb([64, NCH, 64], BF16, f"ATs{d}") for d in range(2)]
        Sdb = [k.sb([128, NCH, 128], BF16, f"Sdb{d}") for d in range(2)]
        rs = k.sb([64, 3, NCH], F32, "rs")
        Oacc = qf.t[0:64, :].bitcast(BF16).rearrange("p (c v) -> p c v", v=128)
        mixh = khT[0]

        def load_wh(h):
            for g in range(5):
                c0 = g * 1024 + h * 128
                k.dma("pool", out=wh1[:, :, g, :], in_=w_in_d[:, c0:c0 + 128].rearrange("(kc p) n -> p kc n", p=128),
                      writes=[wh1])

        def proj_fm(g, evac):
            for tb in range(4):
                pb = PS[tb]
                for kc in range(16):
                    k.op("pe", "matmul", pb[:, :], lhsT=wh1[:, kc, g, :], rhs=hT[:, kc, tb * 512:(tb + 1) * 512],
                         start=(kc == 0), stop=(kc == 15), reads=[wh1, hT], writes=[pb], sig=(kc == 15))
                evac(tb, pb)

        def gate_math(d, dh):
            k.op("act", "activation", tB[:], tA[:], AF.Ln, bias=lbv[:, 0, dh:dh + 1], scale=lbv[:, 1, dh:dh + 1],
                 reads=[tA, lbv], writes=[tB])
            k.op("dve", "tensor_scalar", tA[:], tA[:], lbv[:, 2, dh:dh + 1], lbv[:, 1, dh:dh + 1], ALU.mult, ALU.add,
                 reads=[tA, lbv], writes=[tA])
            k.op("dve", "tensor_tensor_scan", tC[:], scanm[:], tB[:], 0.0, ALU.mult, ALU.add,
                 reads=[scanm, tB], writes=[tC])
            c3 = tC[:].rearrange("p (c l) -> p c l", l=L)
            b3 = tB[:].rearrange("p (c l) -> p c l", l=L)
            k.op("act", "activation", dec[d][:], c3[:, :, L - 1], AF.Exp, reads=[tC], writes=[dec[d]])
            if d == 0:
                k.op("dve", "tensor_tensor", b3, c3[:, :, L - 1:L].to_broadcast([128, NCH, L]), c3, ALU.subtract,
                     reads=[tC], writes=[tB])
            else:
                k.op("dve", "tensor_tensor", tB[:], tC[:], tB[:], ALU.subtract, reads=[tC, tB], writes=[tB])
                for j in range(NCH):
                    k.op("pool", "tensor_copy", decj[1][:, j:j + 1], dec[1][:, NCH - 1 - j:NCH - j], reads=[dec[1]],
                         writes=[decj[1]])
            k.op("act", "activation", tC[:], tB[:], AF.Exp, reads=[tB], writes=[tC])
            k.op("dve", "tensor_tensor", khT[d][:], tA[:], tC[:], ALU.mult, reads=[tA, tC], writes=[khT[d]])
            k.op("act", "activation", tB[:], tB[:], AF.Exp, scale=-1.0, reads=[tB], writes=[tB])
            k.op("dve", "tensor_tensor", qtT[d][:], qf[:], tB[:], ALU.mult, reads=[qf, tB], writes=[qtT[d]])

        def chunk_of(d, j):
            return j if d == 0 else NCH - 1 - j

        def batched(d):
            for g in range(4):
                bank = 4 + g % 2
                for i in range(8):
                    c = chunk_of(d, g * 8 + i)
                    k.op("pe", "transpose", PSB[bank][0:64, i * 128:(i + 1) * 128], khT[d][:, c * L:(c + 1) * L], identb[:],
                         reads=[khT[d], identb], writes=[PS[bank]], sig=(i == 7))
                k.op("act", "copy", khTok[:, g * 8:(g + 1) * 8, :], PSB[bank][0:64, 0:1024].rearrange("p (a n) -> p a n", a=8),
                     reads=[PS[bank]], writes=[khTok])
            for g in range(4):
                bank = 6 + g % 2
                for i in range(8):
                    c = chunk_of(d, g * 8 + i)
                    cs = slice(c * L, (c + 1) * L)
                    k.op("pe", "matmul", PS[bank][0:64, i * 64:(i + 1) * 64], lhsT=khT[d][:, cs], rhs=qtT[d][:, cs], start=True,
                         stop=True, reads=[khT[d], qtT[d]], writes=[PS[bank]], sig=(i == 7))
                k.op("dve", "tensor_tensor", ATs[d][:, g * 8:(g + 1) * 8, :],
                     PS[bank][0:64, :].rearrange("p (a n) -> p a n", a=8),
                     masks[d][:].unsqueeze(1).to_broadcast([64, 8, 64]), ALU.mult, reads=[PS[bank], masks[d]], writes=[ATs[d]])
            U3 = Uall.rearrange("p (v j) -> p v j", j=NCH)
            for g in range(8):
                bank = 4 + g % 2
                for i in range(4):
                    j = g * 4 + i
                    c = chunk_of(d, j)
                    k.op("pe", "matmul", PS[bank][:, i * 128:(i + 1) * 128], lhsT=khTok[:, j, :], rhs=Vt[:, c, :], start=True,
                         stop=True, reads=[khTok, Vt], writes=[PS[bank]], sig=(i == 3))
                k.op("act", "copy", U3[:, :, g * 4:(g + 1) * 4].rearrange("p v j -> p j v"),
                     PS[bank][:, :].rearrange("p (j v) -> p j v", j=4), reads=[PS[bank]], writes=[tA, tB])
            k.op("dve", "tensor_copy", decz[:], decj[d][:], reads=[decj[d]], writes=[decz])
            k.op("dve", "memset", decz[:, 0:1], 0.0, writes=[decz])
            k.op("dve", "tensor_copy", decrep.rearrange("p (v j) -> p v j", j=NCH),
                 decz[:].unsqueeze(1).to_broadcast([128, 32, NCH]), reads=[decz], writes=[tE])
            for qv in range(4):
                sl = slice(qv * 1024, (qv + 1) * 1024)
                k.op("dve", "tensor_tensor_scan", Sall[:, sl], decrep, Uall[:, sl], 0.0, ALU.mult, ALU.add,
                     reads=[tE, tA, tB], writes=[tC, tD])
            S3 = Sall.rearrange("p (v j) -> p v j", j=NCH)
            k.op("dve", "tensor_tensor", Sdb[d][:, 1:NCH, :].rearrange("p j v -> p v j"), S3[:, :, 0:NCH - 1],
                 decj[d][:, 1:NCH].unsqueeze(1).to_broadcast([128, 128, NCH - 1]), ALU.mult,
                 reads=[tC, tD, decj[d]], writes=[Sdb[d]])

        load_wh(0)
        for h in range(8):
            proj_fm(0, lambda tb, pb: k.op("act", "copy", qf[:, tb * 512:(tb + 1) * 512], pb[:, :], reads=[pb], writes=[qf]))
            proj_fm(1, lambda tb, pb: k.op("act", "activation", tA[:, tb * 512:(tb + 1) * 512], pb[:, :], AF.Sigmoid,
                                           reads=[pb], writes=[tA]))
            gate_math(0, h)
            proj_fm(3, lambda tb, pb: k.op("act", "copy", VT[:, tb * 512:(tb + 1) * 512], pb[:, :], reads=[pb], writes=[VT]))
            proj_fm(2, lambda tb, pb: k.op("act", "activation", tA[:, tb * 512:(tb + 1) * 512], pb[:, :], AF.Sigmoid,
                                           reads=[pb], writes=[tA]))
            gate_math(1, 8 + h)
            proj_fm(4, lambda tb, pb: k.op("act", "activation", sgT[:, tb * 512:(tb + 1) * 512], pb[:, :], AF.Silu,
                                           reads=[pb], writes=[sgT]))
            if h + 1 < 8:
                load_wh(h + 1)
            for g in range(4):
                bank = 4 + g % 2
                for i in range(8):
                    c = g * 8 + i
                    k.op("pe", "transpose", PSB[bank][0:64, i * 128:(i + 1) * 128], VT[:, c * L:(c + 1) * L], identb[:],
                         reads=[VT, identb], writes=[PS[bank]], sig=(i == 7))
                k.op("act", "copy", Vt[:, g * 8:(g + 1) * 8, :], PSB[bank][0:64, 0:1024].rearrange("p (a n) -> p a n", a=8),
                     reads=[PS[bank]], writes=[Vt])
            batched(0)
            batched(1)
            for g in range(8):
                bank = 6 + g % 2
                for i in range(4):
                    c = g * 4 + i
                    cs = slice(c * L, (c + 1) * L)
                    o_ps = PS[bank][0:64, i * 128:(i + 1) * 128]
                    mms = [(ATs[0][:, c, :], Vt[:, c, :]), (ATs[1][:, NCH - 1 - c, :], Vt[:, c, :])]
                    if c > 0:
                        mms.append((qtT[0][:, cs], Sdb[0][:, c, :]))
                    if c < NCH - 1:
                        mms.append((qtT[1][:, cs], Sdb[1][:, NCH - 1 - c, :]))
                    for mi, (l_, r_) in enumerate(mms):
                        k.op("pe", "matmul", o_ps, lhsT=l_, rhs=r_, start=(mi == 0), stop=(mi == len(mms) - 1),
                             reads=[ATs[0], ATs[1], Vt, qtT[0], qtT[1], Sdb[0], Sdb[1]], writes=[PS[bank]],
                             sig=(mi == len(mms) - 1 and i == 3))
                k.op("act", "copy", Oacc[:, g * 4:(g + 1) * 4, :], PS[bank][0:64, :].rearrange("p (a v) -> p a v", a=4),
                     reads=[PS[bank]], writes=[qf])
            j3 = tA[0:64, :].rearrange("p (c v) -> p c v", v=128)
            for half in range(2):
                osl = Oacc[:, half * 16:(half + 1) * 16, :]
                k.op("dve", "tensor_tensor", j3, osl, osl, ALU.mult, reads=[qf], writes=[tA])
                k.op("dve", "reduce_sum", rs[:, 0, half * 16:(half + 1) * 16], j3, AX.X, reads=[tA], writes=[rs])
            rstd_from_ss(rs[:, 0, :], rs[:, 2, :], rs[:, 1, :], 1.0 / 128, EPS, [rs], rs, rs)
            onb = tB[0:64, :].bitcast(BF16)[:, 0:NCH * 128].rearrange("p (c v) -> p c v", v=128)
            k.op("dve", "tensor_tensor", onb, Oacc, rs[:, 2, :].unsqueeze(2).to_broadcast([64, NCH, 128]), ALU.mult,
                 reads=[qf, rs], writes=[tB])
            for half in range(2):
                bank = half
                for ci in range(16):
                    c = half * 16 + ci
                    k.op("pe", "transpose", PSB[bank][:, ci * 64:(ci + 1) * 64], onb[:, c, :], identb[0:64, 0:64],
                         reads=[tB, identb], writes=[PS[bank]], sig=(ci == 15))
                k.op("dve", "scalar_tensor_tensor", mixh[:, half * 1024:(half + 1) * 1024], PSB[bank][:, 0:1024],
                     hog[:, h:h + 1], sgT[:, half * 1024:(half + 1) * 1024], ALU.mult, ALU.mult,
                     reads=[PS[bank], hog, sgT], writes=[mixh])
            k.dma("sp", out=mixT_d[:, :, h, :].rearrange("t p n -> p t n"), in_=mixh[:].rearrange("p (t n) -> p t n", n=128),
                  reads=[mixh], writes=[R_mixT])
        k.barrier()
        k.release(m_hT)
        if stop_after <= 3:
            return nc, k

        m4 = k.mark()
        SCQ = 192.0
        qag = k.sb([128, 512], F32, "qag")
        kvag = k.sb([128, 256], F32, "kvag")
        qhg = k.sb([128, 192], F32, "qhg")
        khg = k.sb([128, 192], F32, "khg")
        invf = k.sb([128, 32], F32, "invf")
        k.dma("sp", out=qag[:], in_=qag_d.partition_broadcast(128), writes=[qag])
        k.dma("sp", out=kvag[:], in_=kvag_d.partition_broadcast(128), writes=[kvag])
        k.dma("sp", out=qhg[:], in_=qhg_d.partition_broadcast(128), writes=[qhg])
        k.dma("sp", out=khg[:], in_=khg_d.partition_broadcast(128), writes=[khg])
        k.dma("sp", out=invf[:], in_=invf_d.partition_broadcast(128), writes=[invf])
        posi = k.sb([128, 16], I32, "posi")
        k.dma("sp", out=posi[:], in_=pos_d, writes=[posi])
        posf = k.sb([128, 16], F32, "posf")
        k.op("dve", "tensor_copy", posf[:], posi[:], reads=[posi], writes=[posf])
        cs_t = k.sb([128, 2, 16, 32], F32, "cs_t")
        rt1 = k.sb([128, 2, 16, 32], F32, "rt1")
        rti = k.sb([128, 2, 16, 32], I32, "rti")
        rt2 = k.sb([128, 2, 16, 32], F32, "rt2")
        for t in range(NT):
            k.op("dve", "tensor_scalar", rt1[:, 1, t, :], invf[:], posf[:, t:t + 1], 0.5, ALU.mult, ALU.add,
                 reads=[invf, posf], writes=[rt1])
        k.op("dve", "tensor_scalar", rt1[:, 0], rt1[:, 1], 0.25, None, ALU.add, reads=[rt1], writes=[rt1])
        k.op("dve", "tensor_copy", rti[:], rt1[:], reads=[rt1], writes=[rti])
        k.op("dve", "tensor_copy", rt2[:], rti[:], reads=[rti], writes=[rt2])
        k.op("dve", "tensor_tensor", rt1[:], rt1[:], rt2[:], ALU.subtract, reads=[rt1, rt2], writes=[rt1])
        k.op("dve", "tensor_single_scalar", rt2[:], rt1[:], 0.5, ALU.is_ge, reads=[rt1], writes=[rt2])
        k.op("dve", "tensor_tensor", rt1[:], rt1[:], rt2[:], ALU.subtract, reads=[rt1, rt2], writes=[rt1])
        k.op("dve", "tensor_single_scalar", rt2[:], rt1[:], -0.5, ALU.is_lt, reads=[rt1], writes=[rt2])
        k.op("dve", "tensor_tensor", rt1[:], rt1[:], rt2[:], ALU.add, reads=[rt1, rt2], writes=[rt1])
        k.op("act", "activation", cs_t[:], rt1[:], AF.Sin, scale=-2.0 * np.pi, reads=[rt1], writes=[cs_t])
        cosb = cs_t[:, 0]
        sinb = cs_t[:, 1]

        cqnT = k.sb([128, 4, S], BF16, "cqnT")
        ckvnT = k.sb([128, 2, S], BF16, "ckvnT")
        kper = k.sb([128, NT, 64], F32, "kper")
        sskpe = k.sb([128, NT], F32, "sskpe")
        ld = [k.sb([128, 832], F32, f"ld{i}") for i in range(2)]
        nb_ = [k.sb([128, 768], BF16, f"nb{i}") for i in range(2)]
        jk = k.sb([128, 512], F32, "jk")
        st4 = [k.sb([128, 8], F32, f"st4{i}") for i in range(2)]
        kg = k.sb([128, NT, 64], F32, "kg")
        for t in range(NT):
            l_, n_, s_ = ld[t % 2], nb_[t % 2], st4[t % 2]
            k.dma("sp", out=l_[:], in_=cqkv_d[t * 128:(t + 1) * 128, :], reads=[R_cqkv], writes=[l_])
            k.op("act", "activation", jk[:, 0:512], l_[:, 0:512], AF.Square, accum_out=s_[:, 0:1], reads=[l_], writes=[jk, s_])
            k.op("act", "activation", jk[:, 0:256], l_[:, 512:768], AF.Square, accum_out=s_[:, 1:2], reads=[l_], writes=[jk, s_])
            k.op("act", "activation", jk[:, 0:64], l_[:, 768:832], AF.Square, accum_out=sskpe[:, t:t + 1], reads=[l_],
                 writes=[jk, sskpe])
            rstd_from_ss(s_[:, 0:1], s_[:, 4:5], s_[:, 2:3], 1.0 / 512, EPS, [s_], s_, s_)
            rstd_from_ss(s_[:, 1:2], s_[:, 5:6], s_[:, 3:4], 1.0 / 256, EPS, [s_], s_, s_)
            k.op("dve", "scalar_tensor_tensor", n_[:, 0:512], l_[:, 0:512], s_[:, 4:5], qag[:], ALU.mult, ALU.mult,
                 reads=[l_, s_, qag], writes=[n_])
            k.op("dve", "scalar_tensor_tensor", n_[:, 512:768], l_[:, 512:768], s_[:, 5:6], kvag[:], ALU.mult, ALU.mult,
                 reads=[l_, s_, kvag], writes=[n_])
            k.op("dve", "tensor_tensor", kg[:, t, :], l_[:, 768:832], khg[:, 128:192], ALU.mult, reads=[l_, khg], writes=[kg])
            bank = t % 2
            transpose_evac(cqnT[:, :, t * 128:(t + 1) * 128], cqnT, [n_[:, i * 128:(i + 1) * 128] for i in range(4)], n_, bank)
            transpose_evac(ckvnT[:, :, t * 128:(t + 1) * 128], ckvnT, [n_[:, 512 + i * 128:512 + (i + 1) * 128] for i in range(2)],
                           n_, 2 + bank)

        def rope(dst1, dst2, x1, x2, cos_, sin_, ta, tb_, reads, wr, shape):
            k.op("dve", "tensor_tensor", ta, x1, cos_, ALU.mult, reads=reads + [cs_t], writes=[rt1])
            k.op("dve", "tensor_tensor", tb_, x2, sin_, ALU.mult, reads=reads + [cs_t], writes=[rt2])
            k.op("dve", "tensor_tensor", dst1, ta, tb_, ALU.subtract, reads=[rt1, rt2], writes=[wr])
            k.op("dve", "tensor_tensor", ta, x2, cos_, ALU.mult, reads=reads + [cs_t], writes=[rt1])
            k.op("dve", "tensor_tensor", tb_, x1, sin_, ALU.mult, reads=reads + [cs_t], writes=[rt2])
            k.op("dve", "tensor_tensor", dst2, ta, tb_, ALU.add, reads=[rt1, rt2], writes=[wr])

        rope(kper[:, :, 0:32], kper[:, :, 32:64], kg[:, :, 0:32], kg[:, :, 32:64], cosb, sinb, rt1[:, 0], rt2[:, 0],
             [kg], kper, None)

        wq = [k.sb([128, 4, 192], BF16, f"wq{i}") for i in range(2)]
        wkv = [k.sb([128, 2, 256], BF16, f"wkv{i}") for i in range(2)]
        raw = k.sb([128, NT, 448], F32, "raw")
        jk2 = k.sb([128, NT, 320], F32, "jk2")
        ssq = k.sb([128, 6, NT], F32, "ssq")
        qb = k.sb([128, NT, 192], BF16, "qb")
        kb = k.sb([128, NT, 192], BF16, "kb")
        Vaug2 = [k.sb([128, NT, 132], BF16, f"Vaug{i}") for i in range(2)]
        QKn2 = [k.sb([128, 2, S], BF16, f"QKn{i}") for i in range(2)]
        QKr2 = [k.sb([64, 2, S], BF16, f"QKr{i}") for i in range(2)]
        PT = [k.sb([128, 512], BF16, f"PT{i}") for i in range(2)]
        ob = k.sb([128, NT, 128], BF16, "ob")
        rsum = k.sb([128, 4], F32, "rsum")
        mixa = k.sb([128, S], BF16, "mixa")
        for i in range(2):
            k.op("pool", "memset", Vaug2[i][:, :, 128:132], 1.0, writes=[Vaug2[i]])

        def load_wqkv(h):
            k.dma("pool", out=wq[h % 2][:], in_=w_uq_d[:, h * 192:(h + 1) * 192].rearrange("(kc p) n -> p kc n", p=128),
                  writes=[wq[h % 2]])
            k.dma("pool", out=wkv[h % 2][:], in_=w_ukv_d[:, h * 256:(h + 1) * 256].rearrange("(kc p) n -> p kc n", p=128),
                  writes=[wkv[h % 2]])

        def prep_gen(h):
            wq_, wkv_ = wq[h % 2], wkv[h % 2]
            Vaug, QKn, QKr = Vaug2[h % 2], QKn2[h % 2], QKr2[h % 2]
            pb = PS[2]
            for t in range(NT):
                for kc in range(4):
                    k.op("pe", "matmul", pb[:, 0:192], lhsT=cqnT[:, kc, t * 128:(t + 1) * 128], rhs=wq_[:, kc, :],
                         start=(kc == 0), stop=(kc == 3), reads=[cqnT, wq_], writes=[pb], sig=False)
                for kc in range(2):
                    k.op("pe", "matmul", pb[:, 192:448], lhsT=ckvnT[:, kc, t * 128:(t + 1) * 128], rhs=wkv_[:, kc, :],
                         start=(kc == 0), stop=(kc == 1), reads=[ckvnT, wkv_], writes=[pb], sig=(kc == 1))
                k.op("act", "copy", raw[:, t, :], pb[:, 0:448], reads=[pb], writes=[raw])
                yield
            k.op("dve", "tensor_tensor", jk2[:], raw[:, :, 0:320], raw[:, :, 0:320], ALU.mult, reads=[raw], writes=[jk2])
            k.op("dve", "reduce_sum", ssq[:, 0, :], jk2[:, :, 0:192], AX.X, reads=[jk2], writes=[ssq])
            k.op("dve", "reduce_sum", ssq[:, 1, :], jk2[:, :, 192:320], AX.X, reads=[jk2], writes=[ssq])
            yield
            rstd_from_ss(ssq[:, 0, :], ssq[:, 4, :], ssq[:, 2, :], 1.0, SCQ * EPS, [ssq], ssq, ssq)
            k.op("dve", "tensor_tensor", ssq[:, 1, :], ssq[:, 1, :], sskpe[:], ALU.add, reads=[ssq, sskpe], writes=[ssq])
            rstd_from_ss(ssq[:, 1, :], ssq[:, 5, :], ssq[:, 3, :], 1.0 / SCQ, EPS, [ssq], ssq, ssq)
            rq_b = ssq[:, 4, :].unsqueeze(2)
            rk_b = ssq[:, 5, :].unsqueeze(2)
            yield
            k.op("dve", "tensor_tensor", raw[:, :, 0:192], raw[:, :, 0:192], rq_b.to_broadcast([128, NT, 192]), ALU.mult,
                 reads=[raw, ssq], writes=[raw])
            yield
            k.op("dve", "tensor_tensor", raw[:, :, 0:192], raw[:, :, 0:192],
                 qhg[:].unsqueeze(1).to_broadcast([128, NT, 192]), ALU.mult, reads=[raw, qhg], writes=[raw])
            yield
            k.op("dve", "tensor_copy", qb[:, :, 0:128], raw[:, :, 0:128], reads=[raw], writes=[qb])
            rope(qb[:, :, 128:160], qb[:, :, 160:192], raw[:, :, 128:160], raw[:, :, 160:192], cosb, sinb, rt1[:, 0],
                 rt2[:, 0], [raw], qb, None)
            yield
            k.op("dve", "tensor_tensor", raw[:, :, 192:320], raw[:, :, 192:320], rk_b.to_broadcast([128, NT, 128]), ALU.mult,
                 reads=[raw, ssq], writes=[raw])
            yield
            k.op("dve", "tensor_tensor", kb[:, :, 0:128], raw[:, :, 192:320],
                 khg[:, 0:128].unsqueeze(1).to_broadcast([128, NT, 128]), ALU.mult, reads=[raw, khg], writes=[kb])
            k.op("dve", "tensor_tensor", kb[:, :, 128:192], kper[:], rk_b.to_broadcast([128, NT, 64]), ALU.mult,
                 reads=[kper, ssq], writes=[kb])
            k.op("act", "copy", Vaug[:, :, 0:128], raw[:, :, 320:448], reads=[raw], writes=[Vaug])
            yield
            bank = 3
            for t in range(NT):
                k.op("pe", "transpose", PSB[bank][:, 0:128], qb[:, t, 0:128], identb[:], reads=[qb, identb], writes=[PS[bank]], sig=False)
                k.op("pe", "transpose", PSB[bank][:, 128:256], kb[:, t, 0:128], identb[:], reads=[kb, identb], writes=[PS[bank]], sig=False)
                k.op("pe", "transpose", PSB[bank][0:64, 256:384], qb[:, t, 128:192], identb[:], reads=[qb, identb], writes=[PS[bank]], sig=False)
                k.op("pe", "transpose", PSB[bank][0:64, 384:512], kb[:, t, 128:192], identb[:], reads=[kb, identb], writes=[PS[bank]])
                k.op("act", "copy", QKn[:, :, t * 128:(t + 1) * 128], PSB[bank][:, 0:256].rearrange("p (a n) -> p a n", a=2),
                     reads=[PS[bank]], writes=[QKn])
                k.op("act", "copy", QKr[:, :, t * 128:(t + 1) * 128], PSB[bank][0:64, 256:512].rearrange("p (a n) -> p a n", a=2),
                     reads=[PS[bank]], writes=[QKr])
                yield
            for wi in range(20):
                k.op("pe", "matmul", PS[2][:, :], lhsT=cqnT[:, 0, 0:128], rhs=cqnT[:, 0, 0:512], start=True, stop=True,
                     reads=[cqnT], writes=[PS[2]], sig=(wi == 19))
            yield

        def attn_gen(h):
            Vaug, QKn, QKr = Vaug2[h % 2], QKn2[h % 2], QKr2[h % 2]
            its = [(qblk, kt) for qblk in range(4) for kt in range(NT)]

            def scores(i):
                qblk, kt = its[i]
                qs = slice(qblk * 512, (qblk + 1) * 512)
                ks = slice(kt * 128, (kt + 1) * 128)
                sb_ = PS[i % 2]
                k.op("pe", "matmul", sb_[:, :], lhsT=QKn[:, 1, ks], rhs=QKn[:, 0, qs], start=True, stop=False,
                     reads=[QKn], writes=[sb_], sig=False)
                k.op("pe", "matmul", sb_[:, :], lhsT=QKr[:, 1, ks], rhs=QKr[:, 0, qs], start=False, stop=True,
                     reads=[QKr], writes=[sb_])
                k.op("act", "activation", PT[i % 2][:], sb_[:, :], AF.Exp, reads=[sb_], writes=[PT[i % 2]])

            scores(0)
            scores(1)
            for i, (qblk, kt) in enumerate(its):
                pt = PT[i % 2]
                for j in range(4):
                    k.op("pe", "matmul", PS[4 + j][:, 0:129], lhsT=pt[:, j * 128:(j + 1) * 128], rhs=Vaug[:, kt, 0:129],
                         start=(kt == 0), stop=(kt == NT - 1), reads=[pt, Vaug], writes=[PS[4 + j]],
                         sig=(j == 3))
                if i + 2 < len(its):
                    scores(i + 2)
                if kt == NT - 1:
                    for j in range(4):
                        qt_ = qblk * 4 + j
                        k.op("dve", "reciprocal", rsum[:, j:j + 1], PS[4 + j][:, 128:129], reads=[PS[4 + j]], writes=[rsum])
                        k.op("dve", "tensor_scalar", ob[:, qt_, :], PS[4 + j][:, 0:128], rsum[:, j:j + 1], None, ALU.mult,
                             reads=[PS[4 + j], rsum], writes=[ob])
                yield
            for half in range(2):
                transpose_evac(mixa[:, half * 1024:(half + 1) * 1024], mixa, [ob[:, half * 8 + i, :] for i in range(8)], ob,
                               3)
                yield
            k.dma("sp", out=mixT_d[:, :, 8 + h, :].rearrange("t p n -> p t n"), in_=mixa[:].rearrange("p (t n) -> p t n", n=128),
                  reads=[mixa], writes=[R_mixT])

        load_wqkv(0)
        for _ in prep_gen(0):
            pass
        for h in range(8):
            pn = None
            if h + 1 < 8:
                load_wqkv(h + 1)
                pn = prep_gen(h + 1)
            for _ in attn_gen(h):
                if pn is not None:
                    next(pn, None)
            if pn is not None:
                for _ in pn:
                    pass
        k.barrier()
        k.release(m4)
        if stop_after <= 4:
            return nc, k

        m5 = k.mark()
        wo = k.sb([128, 16, D], BF16, "wo")
        for nbk in range(4):
            k.dma("pool", out=wo[:, :, nbk * 512:(nbk + 1) * 512],
                  in_=w_out_d[:, nbk * 512:(nbk + 1) * 512].rearrange("(kc p) n -> p kc n", p=128), writes=[wo])
        wr = k.sb([128, 16, 16], BF16, "wr")
        k.dma("pool", out=wr[:], in_=w_rt_d.rearrange("(kc p) n -> p kc n", p=128), writes=[wr])
        g1b = k.sb([128, D], F32, "g1b")
        G2b = k.sb([128, D], F32, "G2b")
        Sh2b = k.sb([128, D], F32, "Sh2b")
        bcast_load(g1b, 2 * D)
        bcast_load(G2b, 4 * D)
        bcast_load(Sh2b, 3 * D)
        affT = k.sb([16, S], F32, "affT")
        mx = [k.sb([128, 16, 128], BF16, f"mx{i}") for i in range(2)]
        xt5 = [k.sb([128, D], F32, f"xt5{i}") for i in range(2)]
        x1 = [k.sb([128, D], F32, f"x1{i}") for i in range(2)]
        tm5 = k.sb([128, D], F32, "tm5")
        jk5 = k.sb([128, D], BF16, "jk5")
        h2b = [k.sb([128, D], BF16, f"h2b{i}") for i in range(2)]
        h2T = k.sb([128, 16, 128], BF16, "h2T")
        st5 = [k.sb([128, 8], F32, f"st5{i}") for i in range(2)]
        lg = [k.sb([128, 16], F32, f"lg{i}") for i in range(2)]
        def s5_front(t):
            ts_ = slice(t * 128, (t + 1) * 128)
            m_, xx, x1_ = mx[t % 2], xt5[t % 2], x1[t % 2]
            k.dma("sp", out=m_[:], in_=mixT_d[t], reads=[R_mixT], writes=[m_])
            k.dma("sp", out=xx[:], in_=x_d[ts_, :], writes=[xx])
            for nbk in range(4):
                pb = PS[nbk]
                cs_ = slice(nbk * 512, (nbk + 1) * 512)
                for kc in range(16):
                    k.op("pe", "matmul", pb[:, :], lhsT=m_[:, kc, :], rhs=wo[:, kc, cs_], start=(kc == 0), stop=(kc == 15),
                         reads=[m_, wo], writes=[pb], sig=(kc == 15))
                k.op("dve", "tensor_tensor", tm5[:, cs_], pb[:, :], g1b[:, cs_], ALU.mult, reads=[pb, g1b], writes=[tm5])
                k.op("dve", "tensor_tensor", x1_[:, cs_], tm5[:, cs_], xx[:, cs_], ALU.add, reads=[tm5, xx], writes=[x1_])
            k.dma("pool", out=out_d[ts_, :], in_=x1_[:], reads=[x1_], writes=[R_out])

        def s5_back(t):
            ts_ = slice(t * 128, (t + 1) * 128)
            x1_, hb_, s_, lg_ = x1[t % 2], h2b[t % 2], st5[t % 2], lg[t % 2]
            k.op("act", "activation", jk5[:], x1_[:], AF.Square, accum_out=s_[:, 0:1], reads=[x1_], writes=[jk5, s_])
            rstd_from_ss(s_[:, 0:1], s_[:, 2:3], s_[:, 1:2], 1.0 / D, EPS, [s_], s_, s_)
            k.op("dve", "scalar_tensor_tensor", tm6[:], x1_[:], s_[:, 2:3], G2b[:], ALU.mult, ALU.mult,
                 reads=[x1_, s_, G2b], writes=[tm6])
            k.op("dve", "tensor_tensor", hb_[:], tm6[:], Sh2b[:], ALU.add, reads=[tm6, Sh2b], writes=[hb_])
            k.dma("pool", out=h2_d[ts_, :], in_=hb_[:], reads=[hb_], writes=[R_h2])
            for half in range(2):
                transpose_evac(h2T[:, half * 8:(half + 1) * 8, :], h2T, [hb_[:, (half * 8 + i) * 128:(half * 8 + i + 1) * 128]
                                                                       for i in range(8)], hb_, 4 + half)
            pl = PS[6]
            for kc in range(16):
                k.op("pe", "matmul", pl[:, 0:16], lhsT=h2T[:, kc, :], rhs=wr[:, kc, :], start=(kc == 0), stop=(kc == 15),
                     reads=[h2T, wr], writes=[pl], sig=(kc == 15))
            k.op("dve", "reduce_max", s_[:, 3:4], pl[:, 0:16], AX.X, reads=[pl], writes=[s_])
            k.op("dve", "tensor_scalar", s_[:, 4:5], s_[:, 3:4], -1.0, None, ALU.mult, reads=[s_], writes=[s_])
            k.op("act", "activation", lg_[:], pl[:, 0:16], AF.Exp, bias=s_[:, 4:5], accum_out=s_[:, 5:6], reads=[pl, s_],
                 writes=[lg_, s_])
            k.op("dve", "reciprocal", s_[:, 6:7], s_[:, 5:6], reads=[s_], writes=[s_])
            k.op("dve", "tensor_scalar", lg_[:], lg_[:], s_[:, 6:7], None, ALU.mult, reads=[lg_, s_], writes=[lg_])
            pt_ = PS[7]
            k.op("pe", "transpose", pt_[0:16, 0:128], lg_[:], identf[:], reads=[lg_, identf], writes=[pt_])
            k.op("act", "copy", affT[:, ts_], pt_[0:16, 0:128], reads=[pt_], writes=[affT])

        tm6 = k.sb([128, D], F32, "tm6")
        s5_front(0)
        for t in range(NT):
            if t + 1 < NT:
                s5_front(t + 1)
            s5_back(t)
        if debug:
            k.dma("sp", out=aff_d, in_=affT[:], reads=[affT], writes=[R_aff])
        k.barrier()
        if stop_after <= 5:
            return nc, k

        CAP = 256
        wk_ = k.sb([16, S], F32, "wk")
        vals = k.sb([16, CAP], F32, "vals")
        idxu = k.sb([16, CAP], U32, "idxu")
        idxf = k.sb([16, CAP], F32, "idxf")
        k.op("dve", "tensor_copy", wk_[:], affT[:], reads=[affT], writes=[wk_])
        for r in range(CAP // 8):
            sl = slice(r * 8, (r + 1) * 8)
            k.op("dve", "max", vals[:, sl], wk_[:], reads=[wk_], writes=[vals])
            k.op("dve", "max_index", idxu[:, sl], vals[:, sl], wk_[:], reads=[wk_, vals], writes=[idxu])
            if r < CAP // 8 - 1:
                k.op("dve", "match_replace", wk_[:], vals[:, sl], wk_[:], -1.0, reads=[wk_, vals], writes=[wk_])
        k.op("dve", "tensor_copy", idxf[:], idxu[:], reads=[idxu], writes=[idxf])
        idxT = k.sb([128, 2, 16], I32, "idxT")
        gateT = k.sb([128, 2, 16], F32, "gateT")
        for hf in range(2):
            pa = PS[hf]
            k.op("pe", "transpose", pa[:, 0:16], idxf[:, hf * 128:(hf + 1) * 128], identf[0:16, 0:16], reads=[idxf, identf],
                 writes=[pa], sig=False)
            k.op("pe", "transpose", pa[:, 16:32], vals[:, hf * 128:(hf + 1) * 128], identf[0:16, 0:16], reads=[vals, identf],
                 writes=[pa])
            k.op("dve", "tensor_copy", idxT[:, hf, :], pa[:, 0:16], reads=[pa], writes=[idxT])
            k.op("dve", "tensor_copy", gateT[:, hf, :], pa[:, 16:32], reads=[pa], writes=[gateT])
        k.barrier()
        k.release(m5)
        idxT2 = k.sb([128, 2, 16], I32, "idxT2")
        gateT2 = k.sb([128, 2, 16], F32, "gateT2")
        k.op("dve", "tensor_copy", idxT2[:], idxT[:], reads=[idxT], writes=[idxT2])
        k.op("dve", "tensor_copy", gateT2[:], gateT[:], reads=[gateT], writes=[gateT2])
        k.barrier()
        if stop_after <= 6:
            return nc, k

        g2b = k.sb([128, D], F32, "g2b")
        bcast_load(g2b, 5 * D)
        xe = [k.sb([128, D], BF16, f"xe{i}") for i in range(2)]
        xeT = k.sb([128, 16, CAP], BF16, "xeT")
        wg = [k.sb([128, 16, 512], BF16, f"wg{i}") for i in range(2)]
        wu = [k.sb([128, 16, 512], BF16, f"wu{i}") for i in range(2)]
        wd = [k.sb([128, 16, 512], BF16, f"wd{i}") for i in range(2)]
        hTe = k.sb([128, 16, CAP], BF16, "hTe")
        sa = [k.sb([128, CAP], F32, f"sa{i}") for i in range(2)]
        yst = [k.sb([128, D], F32, f"yst{i}") for i in range(2)]
        gu_cnt = [0]
        gu_buf = {}
        wd_buf = {}

        def gather(e):
            for hf in range(2):
                k.dma("pool", meth="indirect_dma_start", out=xe[hf][:], out_offset=None, in_=h2_d,
                      in_offset=bass.IndirectOffsetOnAxis(ap=idxT2[:, hf, e:e + 1], axis=0), reads=[idxT2, R_h2],
                      writes=[xe[hf]])

        def load_gu(e, fb):
            wg_, wu_ = wg[gu_cnt[0] % 2], wu[gu_cnt[0] % 2]
            gu_cnt[0] += 1
            fsl = slice(fb * 512, (fb + 1) * 512)
            k.dma("pool", out=wg_[:], in_=w_gate_d[e, :, fsl].rearrange("(kc p) n -> p kc n", p=128), writes=[wg_])
            k.dma("pool", out=wu_[:], in_=w_up_d[e, :, fsl].rearrange("(kc p) n -> p kc n", p=128), writes=[wu_])
            gu_buf[(e, fb)] = (wg_, wu_)

        def load_wd(e, db):
            wd_ = wd[db % 2]
            dsl = slice(db * 512, (db + 1) * 512)
            k.dma("pool", out=wd_[:], in_=w_down_d[e, :, dsl].rearrange("(fc p) n -> p fc n", p=128), writes=[wd_])
            wd_buf[(e, db)] = wd_

        gather(0)
        load_gu(0, 0)
        for e in range(16):
            for hf in range(2):
                for half in range(2):
                    transpose_evac(xeT[:, half * 8:(half + 1) * 8, hf * 128:(hf + 1) * 128], xeT,
                                   [xe[hf][:, (half * 8 + i) * 128:(half * 8 + i + 1) * 128] for i in range(8)], xe[hf],
                                   6 + half)
            for fb in range(4):
                if fb + 1 < 4:
                    load_gu(e, fb + 1)
                else:
                    load_wd(e, 0)
                wg_, wu_ = gu_buf.pop((e, fb))
                for fs in range(4):
                    pa, pu = PS[(fs % 2) * 2], PS[(fs % 2) * 2 + 1]
                    for kc in range(16):
                        k.op("pe", "matmul", pa[:, 0:CAP], lhsT=wg_[:, kc, fs * 128:(fs + 1) * 128], rhs=xeT[:, kc, :],
                             start=(kc == 0), stop=(kc == 15), reads=[wg_, xeT], writes=[pa], sig=(kc == 15))
                    for kc in range(16):
                        k.op("pe", "matmul", pu[:, 0:CAP], lhsT=wu_[:, kc, fs * 128:(fs + 1) * 128], rhs=xeT[:, kc, :],
                             start=(kc == 0), stop=(kc == 15), reads=[wu_, xeT], writes=[pu], sig=(kc == 15))
                    s_ = sa[fs % 2]
                    k.op("act", "activation", s_[:], pa[:, 0:CAP], AF.Silu, reads=[pa], writes=[s_])
                    k.op("dve", "tensor_tensor", hTe[:, fb * 4 + fs, :], s_[:], pu[:, 0:CAP], ALU.mult, reads=[s_, pu],
                         writes=[hTe])
            for db in range(4):
                if db + 1 < 4:
                    load_wd(e, db + 1)
                elif e + 1 < 16:
                    gather(e + 1)
                    load_gu(e + 1, 0)
                wd_ = wd_buf.pop((e, db))
                dsl = slice(db * 512, (db + 1) * 512)
                for hf in range(2):
                    py = PS[4 + hf]
                    for fc in range(16):
                        k.op("pe", "matmul", py[:, :], lhsT=hTe[:, fc, hf * 128:(hf + 1) * 128], rhs=wd_[:, fc, :],
                             start=(fc == 0), stop=(fc == 15), reads=[hTe, wd_], writes=[py], sig=(fc == 15))
                    k.op("dve", "scalar_tensor_tensor", yst[hf][:, dsl], py[:, :], gateT2[:, hf, e:e + 1], g2b[:, dsl],
                         ALU.mult, ALU.mult, reads=[py, gateT2, g2b], writes=[yst[hf]])
            for hf in range(2):
                k.dma("pool", meth="indirect_dma_start", out=out_d, out_offset=bass.IndirectOffsetOnAxis(
                    ap=idxT2[:, hf, e:e + 1], axis=0), in_=yst[hf][:], in_offset=None, compute_op=ALU.add,
                    reads=[idxT2, yst[hf], R_out], writes=[R_out])
        k.barrier()
        return nc, k


_CACHE = {}


def _prep(inputs, b):
    f = lambda a: np.ascontiguousarray(a, dtype=np.float32)
    d = {}
    d["x"] = f(inputs["x"][b])
    d["c_t"] = f(inputs["c"][b].reshape(16, 128).T)
    d["pos_t"] = np.ascontiguousarray(inputs["positions"][b].reshape(16, 128).T.astype(np.int32))
    d["w_ada"] = f(inputs["w_ada"][0])
    d["b_ada"] = f(inputs["b_ada"][0].reshape(1, -1))
    d["norm1_g"] = f(inputs["norm1_g"][0].reshape(1, -1))
    d["w_in"] = f(inputs["w_in"][0])
    d["lbl"] = f(inputs["lb_logits"].reshape(2, 2, 8, 128).transpose(3, 0, 1, 2).reshape(128, 32))
    d["hog"] = f(inputs["hgrn_out_g"][0].T)
    d["qa_g"] = f(inputs["qa_norm_g"][0].reshape(1, -1))
    d["w_uq"] = f(inputs["w_uq"][0])
    d["kva_g"] = f(inputs["kva_norm_g"][0].reshape(1, -1))
    d["w_ukv"] = f(inputs["w_ukv"][0])
    d["q_hg"] = f(inputs["q_head_g"][0].reshape(1, -1))
    d["k_hg"] = f(inputs["k_head_g"][0].reshape(1, -1))
    d["w_out"] = f(inputs["w_out"][0])
    d["norm2_g"] = f(inputs["norm2_g"][0].reshape(1, -1))
    d["w_router"] = f(inputs["w_router"][0])
    d["w_gate"] = f(inputs["w_gate"][0])
    d["w_up"] = f(inputs["w_up"][0])
    d["w_down"] = f(inputs["w_down"][0])
    invf = 1.0 / (10000.0 ** (np.arange(0, 64, 2, dtype=np.float32) / 64.0))
    d["inv_freq"] = (invf / np.float32(2 * np.pi)).astype(np.float32).reshape(1, 32)
    return d


def kernel(**inputs):
    inputs = {k_: np.asarray(v) for k_, v in inputs.items()}
    if "nc" not in _CACHE:
        nc, kk = build()
        kk.replay()
        _CACHE["nc"] = nc
    nc = _CACHE["nc"]
    shared = None
    in_maps = []
    for core in range(8):
        b = core % 4
        if core < 4:
            in_maps.append(_prep(inputs, b))
        else:
            in_maps.append(in_maps[b])
    res = run_bass_kernel_spmd(nc, in_maps, core_ids=list(range(8)))
    out = np.stack([np.asarray(res.results[b]["out"], dtype=np.float32) for b in range(4)], axis=0)
    return out
```

```python
import contextlib
import numpy as np
import concourse.bass as bass
import concourse.mybir as mybir
from concourse.bass_utils import run_bass_kernel_spmd

F32 = mybir.dt.float32
BF16 = mybir.dt.bfloat16
I32 = mybir.dt.int32
U32 = mybir.dt.uint32
AF = mybir.ActivationFunctionType
ALU = mybir.AluOpType
AX = mybir.AxisListType

S = 2048
D = 2048
NT = 16
EPS = 1e-6
SB_BASE = 16640
SB_LIMIT = 229376 - 512


class Reg:
    __slots__ = ("name", "w", "rs")

    def __init__(self, name=""):
        self.name = name
        self.w = {}
        self.rs = {}


class T:
    def __init__(self, t, name=""):
        self.t = t
        self.r = Reg(name)

    def __getitem__(self, idx):
        return self.t[idx]


def _regs(xs):
    out = []
    for x in xs:
        if x is None:
            continue
        out.append(x.r if isinstance(x, T) else x)
    return out


class Eng:
    def __init__(self, key, sem):
        self.key = key
        self.sem = sem
        self.count = 0
        self.ops = []
        self.seen = {}


class K:
    def __init__(self, nc, sems):
        self.nc = nc
        self.sems = sems
        self.eng = {k: Eng(k, sems[k]) for k in ("pe", "act", "dve", "pool", "sp")}
        self.dma_pool = {q: [k for k in sems if k.startswith("d" + q)] for q in ("sp", "pool")}
        self.dma_count = {k: 0 for q in self.dma_pool for k in self.dma_pool[q]}
        self.sb_off = SB_BASE
        self.nid = 0

    def sb(self, shape, dtype, name="t"):
        esz = {F32: 4, BF16: 2, I32: 4, U32: 4}[dtype]
        n = 1
        for s in shape[1:]:
            n *= s
        nbytes = (n * esz + 63) // 64 * 64
        off = self.sb_off
        self.sb_off += nbytes
        assert self.sb_off <= SB_LIMIT, f"SBUF overflow at {name}: {self.sb_off}"
        self.nid += 1
        t = self.nc.alloc_sbuf_tensor_at(f"{name}_{self.nid}", list(shape), dtype, offset=off)
        return T(t, name)

    def mark(self):
        return self.sb_off

    def release(self, m):
        self.sb_off = m

    def _need(self, e, toks):
        eng = self.eng[e]
        best = {}
        for t in toks:
            if t is None:
                continue
            sk, v = t
            if best.get(sk, 0) < v:
                best[sk] = v
        for sk, v in best.items():
            if eng.seen.get(sk, 0) >= v:
                continue
            eng.seen[sk] = v
            sem = self.sems[sk]
            eng.ops.append(lambda h, sem=sem, v=v: h.wait_ge(sem, v))

    def _deps(self, e, reads, writes):
        toks = []
        for r in reads:
            for sk, v in r.w.items():
                if sk == e and e == "pe":
                    continue
                toks.append((sk, v))
        for w in writes:
            for sk, v in w.w.items():
                if sk == e and e == "pe":
                    continue
                toks.append((sk, v))
            for sk, v in w.rs.items():
                if sk == e and e == "pe":
                    continue
                toks.append((sk, v))
        self._need(e, toks)

    def _commit(self, tok, reads, writes):
        sk, v = tok
        for r in reads:
            if r.rs.get(sk, 0) < v:
                r.rs[sk] = v
        for w in writes:
            if w.w.get(sk, 0) < v:
                w.w[sk] = v
            w.rs = {}

    def op(self, e, meth, *args, reads=(), writes=(), sig=True, **kw):
        reads = _regs(reads)
        writes = _regs(writes)
        eng = self.eng[e]
        self._deps(e, reads, writes)
        if sig:
            eng.count += 1
            tok = (e, eng.count)
            sem = eng.sem
            eng.ops.append(lambda h: getattr(h, meth)(*args, **kw).then_inc(sem, 1))
        else:
            tok = (e, eng.count + 1)
            eng.ops.append(lambda h: getattr(h, meth)(*args, **kw))
        self._commit(tok, reads, writes)
        return tok

    def dma(self, e, reads=(), writes=(), meth="dma_start", **kw):
        reads = _regs(reads)
        writes = _regs(writes)
        eng = self.eng[e]
        self._deps(e, reads, writes)
        pool = self.dma_pool[e]
        sk = pool.pop(0)
        pool.append(sk)
        if self.dma_count[sk]:
            self._need(e, [(sk, self.dma_count[sk])])
        self.dma_count[sk] += 16
        v = self.dma_count[sk]
        sem = self.sems[sk]
        eng.ops.append(lambda h: getattr(h, meth)(**kw).then_inc(sem, 16))
        tok = (sk, v)
        self._commit(tok, reads, writes)
        return tok

    def barrier(self):
        toks = []
        for kk, eng in self.eng.items():
            if eng.count:
                toks.append((kk, eng.count))
        for sk, v in self.dma_count.items():
            if v:
                toks.append((sk, v))
        for kk in self.eng:
            self._need(kk, toks)

    def replay(self):
        nc = self.nc
        with nc.Block() as block:
            @block.tensor
            def _(h):
                for f in self.eng["pe"].ops:
                    f(h)

            @block.scalar
            def _(h):
                for f in self.eng["act"].ops:
                    f(h)

            @block.vector
            def _(h):
                for f in self.eng["dve"].ops:
                    f(h)

            @block.gpsimd
            def _(h):
                for f in self.eng["pool"].ops:
                    f(h)

            @block.sync
            def _(h):
                for f in self.eng["sp"].ops:
                    f(h)


def build(stop_after=99, debug=False):
    nc = bass.Bass("TRN2", target_bir_lowering=False)

    def din(name, shape, dt=F32):
        return nc.dram_tensor(name, list(shape), dt, kind="ExternalInput").ap()

    x_d = din("x", [S, D])
    c_d = din("c_t", [128, 16])
    pos_d = din("pos_t", [128, 16], I32)
    w_ada_d = din("w_ada", [D, 6 * D])
    b_ada_d = din("b_ada", [1, 6 * D])
    n1g_d = din("norm1_g", [1, D])
    w_in_d = din("w_in", [D, 5952])
    lbl_d = din("lbl", [128, 32])
    hog_d = din("hog", [128, 8])
    qag_d = din("qa_g", [1, 512])
    w_uq_d = din("w_uq", [512, 1536])
    kvag_d = din("kva_g", [1, 256])
    w_ukv_d = din("w_ukv", [256, 2048])
    qhg_d = din("q_hg", [1, 192])
    khg_d = din("k_hg", [1, 192])
    w_out_d = din("w_out", [D, D])
    n2g_d = din("norm2_g", [1, D])
    w_rt_d = din("w_router", [D, 16])
    w_gate_d = din("w_gate", [16, D, D])
    w_up_d = din("w_up", [16, D, D])
    w_down_d = din("w_down", [16, D, D])
    invf_d = din("inv_freq", [1, 32])
    out_d = nc.dram_tensor("out", [S, D], F32, kind="ExternalOutput").ap()
    dbg_kind = "ExternalOutput" if debug else "Internal"
    mod_d = nc.dram_tensor("mod_s", [1, 6 * D], F32, kind=dbg_kind).ap()
    cqkv_d = nc.dram_tensor("cqkv_s", [S, 832], F32, kind=dbg_kind).ap()
    mixT_d = nc.dram_tensor("mixT_s", [NT, 128, 16, 128], BF16, kind=dbg_kind).ap()
    h2_d = nc.dram_tensor("h2_s", [S, D], BF16, kind=dbg_kind).ap()
    aff_d = nc.dram_tensor("aff_s", [16, S], F32, kind=dbg_kind).ap()
    R_mod, R_cqkv, R_mixT, R_h2, R_out = Reg("mod_d"), Reg("cqkv_d"), Reg("mixT_d"), Reg("h2_d"), Reg("out_d")
    R_aff = Reg("aff_d")

    names = ["pe", "act", "dve", "pool", "sp"] + [f"dsp{i}" for i in range(24)] + [f"dpool{i}" for i in range(16)]
    with contextlib.ExitStack() as es:
        sems = {n: nc.alloc_semaphore(n) for n in names}
        k = K(nc, sems)
        PS = [T(nc.alloc_psum_tensor(f"psb{i}", [128, 512], F32), f"ps{i}") for i in range(8)]
        PSB = [p.t[:].bitcast(BF16) for p in PS]

        identb = k.sb([128, 128], BF16, "identb")
        identf = k.sb([128, 128], F32, "identf")
        for idt in (identb, identf):
            k.op("pool", "memset", idt[:], 0.0, writes=[idt])
            k.op("pool", "affine_select", idt[:], idt[:], pattern=[[-1, 128]], compare_op=ALU.not_equal,
                 fill=1.0, base=0, channel_multiplier=1, reads=[idt], writes=[idt])

        def rstd_from_ss(ss_ap, out_ap, tmp_ap, scale, eps, reads, writes_t, tmp_t):
            k.op("dve", "tensor_scalar", tmp_ap, ss_ap, scale, eps, ALU.mult, ALU.add, reads=reads, writes=[tmp_t])
            k.op("act", "activation", tmp_ap, tmp_ap, AF.Sqrt, reads=[tmp_t], writes=[tmp_t])
            k.op("dve", "reciprocal", out_ap, tmp_ap, reads=[tmp_t], writes=[writes_t])

        def transpose_evac(dst_ap, dst_t, src_blocks, src_t, bank, parts_out=128, ncols=128, eng="act"):
            n = len(src_blocks)
            for i, blk in enumerate(src_blocks):
                pin = blk.shape[0]
                k.op("pe", "transpose", PSB[bank][0:parts_out, i * ncols:(i + 1) * ncols], blk, identb[0:pin, 0:pin],
                     reads=[src_t, identb], writes=[PS[bank]], sig=(i == n - 1))
            src = PSB[bank][0:parts_out, 0:n * ncols]
            if len(dst_ap.shape) == 3:
                src = src.rearrange("p (a n) -> p a n", a=n)
            if eng == "act":
                k.op("act", "copy", dst_ap, src, reads=[PS[bank]], writes=[dst_t])
            else:
                k.op("dve", "tensor_copy", dst_ap, src, reads=[PS[bank]], writes=[dst_t])

        m_hT = k.mark()
        hT = k.sb([128, 16, S], BF16, "hT")
        m0 = k.mark()
        ct = k.sb([128, 16], F32, "ct")
        k.dma("sp", out=ct[:], in_=c_d, writes=[ct])
        scb = k.sb([128, 16], BF16, "scb")
        k.op("act", "activation", scb[:], ct[:], AF.Silu, reads=[ct], writes=[scb])
        wada = [k.sb([128, 16, 512], BF16, f"wada{i}") for i in range(2)]
        bblk = [k.sb([1, 512], F32, f"bblk{i}") for i in range(2)]
        gblk = [k.sb([1, 512], F32, f"gblk{i}") for i in range(2)]
        mblk = [k.sb([1, 512], F32, f"mblk{i}") for i in range(2)]

        def mod_block(nb):
            wb, bb, gb, mm_ = wada[nb % 2], bblk[nb % 2], gblk[nb % 2], mblk[nb % 2]
            cs_ = slice(nb * 512, (nb + 1) * 512)
            k.dma("pool", out=wb[:], in_=w_ada_d[:, cs_].rearrange("(kc p) n -> p kc n", p=128), writes=[wb])
            k.dma("sp", out=bb[:], in_=b_ada_d[0:1, cs_], writes=[bb])
            gsrc = None
            if 4 <= nb < 8:
                gsrc = n1g_d[0:1, (nb - 4) * 512:(nb - 3) * 512]
            elif 16 <= nb < 20:
                gsrc = n2g_d[0:1, (nb - 16) * 512:(nb - 15) * 512]
            if gsrc is not None:
                k.dma("sp", out=gb[:], in_=gsrc, writes=[gb])
            pb = PS[6 + nb % 2]
            for kc in range(16):
                k.op("pe", "matmul", pb[0:1, :], lhsT=scb[:, kc:kc + 1], rhs=wb[:, kc, :], start=(kc == 0),
                     stop=(kc == 15), reads=[scb, wb], writes=[pb], sig=(kc == 15))
            k.op("dve", "tensor_tensor", mm_[:], pb[0:1, :], bb[:], ALU.add, reads=[pb, bb], writes=[mm_])
            if gsrc is not None:
                k.op("dve", "scalar_tensor_tensor", mm_[:], mm_[:], 1.0, gb[:], ALU.add, ALU.mult, reads=[mm_, gb],
                     writes=[mm_])
            k.dma("sp", out=mod_d[0:1, cs_], in_=mm_[:], reads=[mm_], writes=[R_mod])

        for nb in range(8):
            mod_block(nb)

        def bcast_load(dst, off, n=D):
            k.dma("sp", out=dst[:], in_=mod_d[0:1, off:off + n].partition_broadcast(128), reads=[R_mod], writes=[dst])

        m1 = k.mark()
        G1b = k.sb([128, D], F32, "G1b")
        Sh1b = k.sb([128, D], F32, "Sh1b")
        bcast_load(G1b, D)
        bcast_load(Sh1b, 0)
        xt = [k.sb([128, D], F32, f"xt{i}") for i in range(2)]
        junk = k.sb([128, D], BF16, "junk")
        tmpf = k.sb([128, D], F32, "tmpf")
        hb = [k.sb([128, D], BF16, f"hb{i}") for i in range(2)]
        st = [k.sb([128, 4], F32, f"st{i}") for i in range(2)]
        for t in range(NT):
            xx, hh, ss = xt[t % 2], hb[t % 2], st[t % 2]
            k.dma("sp", out=xx[:], in_=x_d[t * 128:(t + 1) * 128, :], writes=[xx])
            mod_block(8 + t)
            k.op("act", "activation", junk[:], xx[:], AF.Square, accum_out=ss[:, 0:1], reads=[xx], writes=[junk, ss])
            rstd_from_ss(ss[:, 0:1], ss[:, 2:3], ss[:, 1:2], 1.0 / D, EPS, [ss], ss, ss)
            k.op("dve", "scalar_tensor_tensor", tmpf[:], xx[:], ss[:, 2:3], G1b[:], ALU.mult, ALU.mult,
                 reads=[xx, ss, G1b], writes=[tmpf])
            k.op("dve", "tensor_tensor", hh[:], tmpf[:], Sh1b[:], ALU.add, reads=[tmpf, Sh1b], writes=[hh])
            for half in range(2):
                bank = (t % 2) * 2 + half
                blocks = [hh[:, (half * 8 + i) * 128:(half * 8 + i + 1) * 128] for i in range(8)]
                transpose_evac(hT[:, half * 8:(half + 1) * 8, t * 128:(t + 1) * 128], hT, blocks, hh, bank)
        k.barrier()
        k.release(m0)
        if stop_after <= 1:
            return nc, k

        m2 = k.mark()
        wm = k.sb([128, 16, 832], BF16, "wm")
        k.dma("pool", out=wm[:], in_=w_in_d[:, 5120:5952].rearrange("(kc p) n -> p kc n", p=128), writes=[wm])
        stg = [k.sb([128, 832], F32, f"stg{i}") for i in range(2)]
        for t in range(NT):
            sg_ = stg[t % 2]
            for j, (c0, c1) in enumerate(((0, 512), (512, 832))):
                pb = PS[(t % 2) * 2 + j]
                for kc in range(16):
                    k.op("pe", "matmul", pb[:, 0:c1 - c0], lhsT=hT[:, kc, t * 128:(t + 1) * 128], rhs=wm[:, kc, c0:c1],
                         start=(kc == 0), stop=(kc == 15), reads=[hT, wm], writes=[pb], sig=(kc == 15))
                k.op("act", "copy", sg_[:, c0:c1], pb[:, 0:c1 - c0], reads=[pb], writes=[sg_])
            k.dma("sp", out=cqkv_d[t * 128:(t + 1) * 128, :], in_=sg_[:], reads=[sg_], writes=[R_cqkv])
        k.barrier()
        k.release(m2)
        if stop_after <= 2:
            return nc, k

        m3 = k.mark()
        L = 64
        NCH = S // L
        maskf = k.sb([64, 64], F32, "maskf")
        maskb = k.sb([64, 64], F32, "maskb")
        k.op("pool", "memset", maskf[:], 1.0, writes=[maskf])
        k.op("pool", "affine_select", maskf[:], maskf[:], pattern=[[1, 64]], compare_op=ALU.is_ge, fill=0.0, base=0,
             channel_multiplier=-1, reads=[maskf], writes=[maskf])
        k.op("pool", "memset", maskb[:], 1.0, writes=[maskb])
        k.op("pool", "affine_select", maskb[:], maskb[:], pattern=[[-1, 64]], compare_op=ALU.is_ge, fill=0.0, base=0,
             channel_multiplier=1, reads=[maskb], writes=[maskb])
        masks = (maskf, maskb)
        scanm = k.sb([128, S], BF16, "scanm")
        k.op("pool", "memset", scanm[:], 1.0, writes=[scanm])
        k.op("pool", "memset", scanm[:].rearrange("p (c l) -> p c l", l=L)[:, :, 0:1], 0.0, writes=[scanm])
        lbl = k.sb([128, 32], F32, "lbl")
        k.dma("sp", out=lbl[:], in_=lbl_d, writes=[lbl])
        lbv = k.sb([128, 3, 16], F32, "lbv")
        l4 = lbl[:].rearrange("p (d s h) -> p d s h", d=2, s=2)
        lb_dh = lbv[:, 0, :].rearrange("p (d h) -> p d h", d=2)
        k.op("dve", "tensor_tensor", lb_dh, l4[:, :, 0, :], l4[:, :, 1, :], ALU.subtract, reads=[lbl], writes=[lbv])
        k.op("act", "activation", lbv[:, 0, :], lbv[:, 0, :], AF.Sigmoid, reads=[lbv], writes=[lbv])
        k.op("dve", "tensor_scalar", lbv[:, 1, :], lbv[:, 0, :], -1.0, 1.0, ALU.mult, ALU.add, reads=[lbv], writes=[lbv])
        k.op("dve", "tensor_scalar", lbv[:, 2, :], lbv[:, 0, :], -1.0, None, ALU.add, reads=[lbv], writes=[lbv])
        hog = k.sb([128, 8], F32, "hog")
        k.dma("sp", out=hog[:], in_=hog_d, writes=[hog])

        wh1 = k.sb([128, 16, 5, 128], BF16, "wh")
        qf = k.sb([128, S], F32, "qf")
        scr = k.sb([128, 9216], F32, "scr")
        tA = T(scr.t[:, 0:2048], "tA")
        tB = T(scr.t[:, 2048:4096], "tB")
        tC = T(scr.t[:, 4096:6144], "tC")
        tD = T(scr.t[:, 6144:8192], "tD")
        tE = T(scr.t[:, 8192:9216], "tE")
        Uall = scr.t[:, 0:4096]
        Sall = scr.t[:, 4096:8192]
        decrep = scr.t[:, 8192:9216]
        sgT = k.sb([128, S], BF16, "sgT")
        VT = k.sb([128, S], BF16, "VT")
        khT = [k.sb([128, S], BF16, f"khT{d}") for d in range(2)]
        qtT = [k.sb([128, S], BF16, f"qtT{d}") for d in range(2)]
        dec = [k.sb([128, NCH], F32, f"dec{d}") for d in range(2)]
        decj = [dec[0], k.sb([128, NCH], F32, "decj1")]
        decz = k.sb([128, NCH], F32, "decz")
        Vt = k.sb([64, NCH, 128], BF16, "Vt")
        khTok = k.sb([64, NCH, 128], BF16, "khTok")
        ATs = [k.sb([64, NCH, 64], BF16, f"ATs{d}") for d in range(2)]
        Sdb = [k.sb([128, NCH, 128], BF16, f"Sdb{d}") for d in range(2)]
        rs = k.sb([64, 3, NCH], F32, "rs")
        Oacc = qf.t[0:64, :].bitcast(BF16).rearrange("p (c v) -> p c v", v=128)
        mixh = khT[0]

        def load_wh(h):
            for g in range(5):
                c0 = g * 1024 + h * 128
                k.dma("pool", out=wh1[:, :, g, :], in_=w_in_d[:, c0:c0 + 128].rearrange("(kc p) n -> p kc n", p=128),
                      writes=[wh1])

        def proj_fm(g, evac):
            for tb in range(4):
                pb = PS[tb]
                for kc in range(16):
                    k.op("pe", "matmul", pb[:, :], lhsT=wh1[:, kc, g, :], rhs=hT[:, kc, tb * 512:(tb + 1) * 512],
                         start=(kc == 0), stop=(kc == 15), reads=[wh1, hT], writes=[pb], sig=(kc == 15))
                evac(tb, pb)

        def gate_math(d, dh):
            k.op("act", "activation", tB[:], tA[:], AF.Ln, bias=lbv[:, 0, dh:dh + 1], scale=lbv[:, 1, dh:dh + 1],
                 reads=[tA, lbv], writes=[tB])
            k.op("dve", "tensor_scalar", tA[:], tA[:], lbv[:, 2, dh:dh + 1], lbv[:, 1, dh:dh + 1], ALU.mult, ALU.add,
                 reads=[tA, lbv], writes=[tA])
            k.op("dve", "tensor_tensor_scan", tC[:], scanm[:], tB[:], 0.0, ALU.mult, ALU.add,
                 reads=[scanm, tB], writes=[tC])
            c3 = tC[:].rearrange("p (c l) -> p c l", l=L)
            b3 = tB[:].rearrange("p (c l) -> p c l", l=L)
            k.op("act", "activation", dec[d][:], c3[:, :, L - 1], AF.Exp, reads=[tC], writes=[dec[d]])
            if d == 0:
                k.op("dve", "tensor_tensor", b3, c3[:, :, L - 1:L].to_broadcast([128, NCH, L]), c3, ALU.subtract,
                     reads=[tC], writes=[tB])
            else:
                k.op("dve", "tensor_tensor", tB[:], tC[:], tB[:], ALU.subtract, reads=[tC, tB], writes=[tB])
                for j in range(NCH):
                    k.op("pool", "tensor_copy", decj[1][:, j:j + 1], dec[1][:, NCH - 1 - j:NCH - j], reads=[dec[1]],
                         writes=[decj[1]])
            k.op("act", "activation", tC[:], tB[:], AF.Exp, reads=[tB], writes=[tC])
            k.op("dve", "tensor_tensor", khT[d][:], tA[:], tC[:], ALU.mult, reads=[tA, tC], writes=[khT[d]])
            k.op("act", "activation", tB[:], tB[:], AF.Exp, scale=-1.0, reads=[tB], writes=[tB])
            k.op("dve", "tensor_tensor", qtT[d][:], qf[:], tB[:], ALU.mult, reads=[qf, tB], writes=[qtT[d]])

        def chunk_of(d, j):
            return j if d == 0 else NCH - 1 - j

        def batched(d):
            for g in range(4):
                bank = 4 + g % 2
                for i in range(8):
                    c = chunk_of(d, g * 8 + i)
                    k.op("pe", "transpose", PSB[bank][0:64, i * 128:(i + 1) * 128], khT[d][:, c * L:(c + 1) * L], identb[:],
                         reads=[khT[d], identb], writes=[PS[bank]], sig=(i == 7))
                k.op("act", "copy", khTok[:, g * 8:(g + 1) * 8, :], PSB[bank][0:64, 0:1024].rearrange("p (a n) -> p a n", a=8),
                     reads=[PS[bank]], writes=[khTok])
            for g in range(4):
                bank = 6 + g % 2
                for i in range(8):
                    c = chunk_of(d, g * 8 + i)
                    cs = slice(c * L, (c + 1) * L)
                    k.op("pe", "matmul", PS[bank][0:64, i * 64:(i + 1) * 64], lhsT=khT[d][:, cs], rhs=qtT[d][:, cs], start=True,
                         stop=True, reads=[khT[d], qtT[d]], writes=[PS[bank]], sig=(i == 7))
                k.op("dve", "tensor_tensor", ATs[d][:, g * 8:(g + 1) * 8, :],
                     PS[bank][0:64, :].rearrange("p (a n) -> p a n", a=8),
                     masks[d][:].unsqueeze(1).to_broadcast([64, 8, 64]), ALU.mult, reads=[PS[bank], masks[d]], writes=[ATs[d]])
            U3 = Uall.rearrange("p (v j) -> p v j", j=NCH)
            for g in range(8):
                bank = 4 + g % 2
                for i in range(4):
                    j = g * 4 + i
                    c = chunk_of(d, j)
                    k.op("pe", "matmul", PS[bank][:, i * 128:(i + 1) * 128], lhsT=khTok[:, j, :], rhs=Vt[:, c, :], start=True,
                         stop=True, reads=[khTok, Vt], writes=[PS[bank]], sig=(i == 3))
                k.op("act", "copy", U3[:, :, g * 4:(g + 1) * 4].rearrange("p v j -> p j v"),
                     PS[bank][:, :].rearrange("p (j v) -> p j v", j=4), reads=[PS[bank]], writes=[tA, tB])
            k.op("dve", "tensor_copy", decz[:], decj[d][:], reads=[decj[d]], writes=[decz])
            k.op("dve", "memset", decz[:, 0:1], 0.0, writes=[decz])
            k.op("dve", "tensor_copy", decrep.rearrange("p (v j) -> p v j", j=NCH),
                 decz[:].unsqueeze(1).to_broadcast([128, 32, NCH]), reads=[decz], writes=[tE])
            for qv in range(4):
                sl = slice(qv * 1024, (qv + 1) * 1024)
                k.op("dve", "tensor_tensor_scan", Sall[:, sl], decrep, Uall[:, sl], 0.0, ALU.mult, ALU.add,
                     reads=[tE, tA, tB], writes=[tC, tD])
            S3 = Sall.rearrange("p (v j) -> p v j", j=NCH)
            k.op("dve", "tensor_tensor", Sdb[d][:, 1:NCH, :].rearrange("p j v -> p v j"), S3[:, :, 0:NCH - 1],
                 decj[d][:, 1:NCH].unsqueeze(1).to_broadcast([128, 128, NCH - 1]), ALU.mult,
                 reads=[tC, tD, decj[d]], writes=[Sdb[d]])

        load_wh(0)
        for h in range(8):
            proj_fm(0, lambda tb, pb: k.op("act", "copy", qf[:, tb * 512:(tb + 1) * 512], pb[:, :], reads=[pb], writes=[qf]))
            proj_fm(1, lambda tb, pb: k.op("act", "activation", tA[:, tb * 512:(tb + 1) * 512], pb[:, :], AF.Sigmoid,
                                           reads=[pb], writes=[tA]))
            gate_math(0, h)
            proj_fm(3, lambda tb, pb: k.op("act", "copy", VT[:, tb * 512:(tb + 1) * 512], pb[:, :], reads=[pb], writes=[VT]))
            proj_fm(2, lambda tb, pb: k.op("act", "activation", tA[:, tb * 512:(tb + 1) * 512], pb[:, :], AF.Sigmoid,
                                           reads=[pb], writes=[tA]))
            gate_math(1, 8 + h)
            proj_fm(4, lambda tb, pb: k.op("act", "activation", sgT[:, tb * 512:(tb + 1) * 512], pb[:, :], AF.Silu,
                                           reads=[pb], writes=[sgT]))
            if h + 1 < 8:
                load_wh(h + 1)
            for g in range(4):
                bank = 4 + g % 2
                for i in range(8):
                    c = g * 8 + i
                    k.op("pe", "transpose", PSB[bank][0:64, i * 128:(i + 1) * 128], VT[:, c * L:(c + 1) * L], identb[:],
                         reads=[VT, identb], writes=[PS[bank]], sig=(i == 7))
                k.op("act", "copy", Vt[:, g * 8:(g + 1) * 8, :], PSB[bank][0:64, 0:1024].rearrange("p (a n) -> p a n", a=8),
                     reads=[PS[bank]], writes=[Vt])
            batched(0)
            batched(1)
            for g in range(8):
                bank = 6 + g % 2
                for i in range(4):
                    c = g * 4 + i
                    cs = slice(c * L, (c + 1) * L)
                    o_ps = PS[bank][0:64, i * 128:(i + 1) * 128]
                    mms = [(ATs[0][:, c, :], Vt[:, c, :]), (ATs[1][:, NCH - 1 - c, :], Vt[:, c, :])]
                    if c > 0:
                        mms.append((qtT[0][:, cs], Sdb[0][:, c, :]))
                    if c < NCH - 1:
                        mms.append((qtT[1][:, cs], Sdb[1][:, NCH - 1 - c, :]))
                    for mi, (l_, r_) in enumerate(mms):
                        k.op("pe", "matmul", o_ps, lhsT=l_, rhs=r_, start=(mi == 0), stop=(mi == len(mms) - 1),
                             reads=[ATs[0], ATs[1], Vt, qtT[0], qtT[1], Sdb[0], Sdb[1]], writes=[PS[bank]],
                             sig=(mi == len(mms) - 1 and i == 3))
                k.op("act", "copy", Oacc[:, g * 4:(g + 1) * 4, :], PS[bank][0:64, :].rearrange("p (a v) -> p a v", a=4),
                     reads=[PS[bank]], writes=[qf])
            j3 = tA[0:64, :].rearrange("p (c v) -> p c v", v=128)
            for half in range(2):
                osl = Oacc[:, half * 16:(half + 1) * 16, :]
                k.op("dve", "tensor_tensor", j3, osl, osl, ALU.mult, reads=[qf], writes=[tA])
                k.op("dve", "reduce_sum", rs[:, 0, half * 16:(half + 1) * 16], j3, AX.X, reads=[tA], writes=[rs])
            rstd_from_ss(rs[:, 0, :], rs[:, 2, :], rs[:, 1, :], 1.0 / 128, EPS, [rs], rs, rs)
            onb = tB[0:64, :].bitcast(BF16)[:, 0:NCH * 128].rearrange("p (c v) -> p c v", v=128)
            k.op("dve", "tensor_tensor", onb, Oacc, rs[:, 2, :].unsqueeze(2).to_broadcast([64, NCH, 128]), ALU.mult,
                 reads=[qf, rs], writes=[tB])
            for half in range(2):
                bank = half
                for ci in range(16):
                    c = half * 16 + ci
                    k.op("pe", "transpose", PSB[bank][:, ci * 64:(ci + 1) * 64], onb[:, c, :], identb[0:64, 0:64],
                         reads=[tB, identb], writes=[PS[bank]], sig=(ci == 15))
                k.op("dve", "scalar_tensor_tensor", mixh[:, half * 1024:(half + 1) * 1024], PSB[bank][:, 0:1024],
                     hog[:, h:h + 1], sgT[:, half * 1024:(half + 1) * 1024], ALU.mult, ALU.mult,
                     reads=[PS[bank], hog, sgT], writes=[mixh])
            k.dma("sp", out=mixT_d[:, :, h, :].rearrange("t p n -> p t n"), in_=mixh[:].rearrange("p (t n) -> p t n", n=128),
                  reads=[mixh], writes=[R_mixT])
        k.barrier()
        k.release(m_hT)
        if stop_after <= 3:
            return nc, k

        m4 = k.mark()
        SCQ = 192.0
        qag = k.sb([128, 512], F32, "qag")
        kvag = k.sb([128, 256], F32, "kvag")
        qhg = k.sb([128, 192], F32, "qhg")
        khg = k.sb([128, 192], F32, "khg")
        invf = k.sb([128, 32], F32, "invf")
        k.dma("sp", out=qag[:], in_=qag_d.partition_broadcast(128), writes=[qag])
        k.dma("sp", out=kvag[:], in_=kvag_d.partition_broadcast(128), writes=[kvag])
        k.dma("sp", out=qhg[:], in_=qhg_d.partition_broadcast(128), writes=[qhg])
        k.dma("sp", out=khg[:], in_=khg_d.partition_broadcast(128), writes=[khg])
        k.dma("sp", out=invf[:], in_=invf_d.partition_broadcast(128), writes=[invf])
        posi = k.sb([128, 16], I32, "posi")
        k.dma("sp", out=posi[:], in_=pos_d, writes=[posi])
        posf = k.sb([128, 16], F32, "posf")
        k.op("dve", "tensor_copy", posf[:], posi[:], reads=[posi], writes=[posf])
        cs_t = k.sb([128, 2, 16, 32], F32, "cs_t")
        rt1 = k.sb([128, 2, 16, 32], F32, "rt1")
        rti = k.sb([128, 2, 16, 32], I32, "rti")
        rt2 = k.sb([128, 2, 16, 32], F32, "rt2")
        for t in range(NT):
            k.op("dve", "tensor_scalar", rt1[:, 1, t, :], invf[:], posf[:, t:t + 1], 0.5, ALU.mult, ALU.add,
                 reads=[invf, posf], writes=[rt1])
        k.op("dve", "tensor_scalar", rt1[:, 0], rt1[:, 1], 0.25, None, ALU.add, reads=[rt1], writes=[rt1])
        k.op("dve", "tensor_copy", rti[:], rt1[:], reads=[rt1], writes=[rti])
        k.op("dve", "tensor_copy", rt2[:], rti[:], reads=[rti], writes=[rt2])
        k.op("dve", "tensor_tensor", rt1[:], rt1[:], rt2[:], ALU.subtract, reads=[rt1, rt2], writes=[rt1])
        k.op("dve", "tensor_single_scalar", rt2[:], rt1[:], 0.5, ALU.is_ge, reads=[rt1], writes=[rt2])
        k.op("dve", "tensor_tensor", rt1[:], rt1[:], rt2[:], ALU.subtract, reads=[rt1, rt2], writes=[rt1])
        k.op("dve", "tensor_single_scalar", rt2[:], rt1[:], -0.5, ALU.is_lt, reads=[rt1], writes=[rt2])
        k.op("dve", "tensor_tensor", rt1[:], rt1[:], rt2[:], ALU.add, reads=[rt1, rt2], writes=[rt1])
        k.op("act", "activation", cs_t[:], rt1[:], AF.Sin, scale=-2.0 * np.pi, reads=[rt1], writes=[cs_t])
        cosb = cs_t[:, 0]
        sinb = cs_t[:, 1]

        cqnT = k.sb([128, 4, S], BF16, "cqnT")
        ckvnT = k.sb([128, 2, S], BF16, "ckvnT")
        kper = k.sb([128, NT, 64], F32, "kper")
        sskpe = k.sb([128, NT], F32, "sskpe")
        ld = [k.sb([128, 832], F32, f"ld{i}") for i in range(2)]
        nb_ = [k.sb([128, 768], BF16, f"nb{i}") for i in range(2)]
        jk = k.sb([128, 512], F32, "jk")
        st4 = [k.sb([128, 8], F32, f"st4{i}") for i in range(2)]
        kg = k.sb([128, NT, 64], F32, "kg")
        for t in range(NT):
            l_, n_, s_ = ld[t % 2], nb_[t % 2], st4[t % 2]
            k.dma("sp", out=l_[:], in_=cqkv_d[t * 128:(t + 1) * 128, :], reads=[R_cqkv], writes=[l_])
            k.op("act", "activation", jk[:, 0:512], l_[:, 0:512], AF.Square, accum_out=s_[:, 0:1], reads=[l_], writes=[jk, s_])
            k.op("act", "activation", jk[:, 0:256], l_[:, 512:768], AF.Square, accum_out=s_[:, 1:2], reads=[l_], writes=[jk, s_])
            k.op("act", "activation", jk[:, 0:64], l_[:, 768:832], AF.Square, accum_out=sskpe[:, t:t + 1], reads=[l_],
                 writes=[jk, sskpe])
            rstd_from_ss(s_[:, 0:1], s_[:, 4:5], s_[:, 2:3], 1.0 / 512, EPS, [s_], s_, s_)
            rstd_from_ss(s_[:, 1:2], s_[:, 5:6], s_[:, 3:4], 1.0 / 256, EPS, [s_], s_, s_)
            k.op("dve", "scalar_tensor_tensor", n_[:, 0:512], l_[:, 0:512], s_[:, 4:5], qag[:], ALU.mult, ALU.mult,
                 reads=[l_, s_, qag], writes=[n_])
            k.op("dve", "scalar_tensor_tensor", n_[:, 512:768], l_[:, 512:768], s_[:, 5:6], kvag[:], ALU.mult, ALU.mult,
                 reads=[l_, s_, kvag], writes=[n_])
            k.op("dve", "tensor_tensor", kg[:, t, :], l_[:, 768:832], khg[:, 128:192], ALU.mult, reads=[l_, khg], writes=[kg])
            bank = t % 2
            transpose_evac(cqnT[:, :, t * 128:(t + 1) * 128], cqnT, [n_[:, i * 128:(i + 1) * 128] for i in range(4)], n_, bank)
            transpose_evac(ckvnT[:, :, t * 128:(t + 1) * 128], ckvnT, [n_[:, 512 + i * 128:512 + (i + 1) * 128] for i in range(2)],
                           n_, 2 + bank)

        def rope(dst1, dst2, x1, x2, cos_, sin_, ta, tb_, reads, wr, shape):
            k.op("dve", "tensor_tensor", ta, x1, cos_, ALU.mult, reads=reads + [cs_t], writes=[rt1])
            k.op("dve", "tensor_tensor", tb_, x2, sin_, ALU.mult, reads=reads + [cs_t], writes=[rt2])
            k.op("dve", "tensor_tensor", dst1, ta, tb_, ALU.subtract, reads=[rt1, rt2], writes=[wr])
            k.op("dve", "tensor_tensor", ta, x2, cos_, ALU.mult, reads=reads + [cs_t], writes=[rt1])
            k.op("dve", "tensor_tensor", tb_, x1, sin_, ALU.mult, reads=reads + [cs_t], writes=[rt2])
            k.op("dve", "tensor_tensor", dst2, ta, tb_, ALU.add, reads=[rt1, rt2], writes=[wr])

        rope(kper[:, :, 0:32], kper[:, :, 32:64], kg[:, :, 0:32], kg[:, :, 32:64], cosb, sinb, rt1[:, 0], rt2[:, 0],
             [kg], kper, None)

        wq = [k.sb([128, 4, 192], BF16, f"wq{i}") for i in range(2)]
        wkv = [k.sb([128, 2, 256], BF16, f"wkv{i}") for i in range(2)]
        raw = k.sb([128, NT, 448], F32, "raw")
        jk2 = k.sb([128, NT, 320], F32, "jk2")
        ssq = k.sb([128, 6, NT], F32, "ssq")
        qb = k.sb([128, NT, 192], BF16, "qb")
        kb = k.sb([128, NT, 192], BF16, "kb")
        Vaug2 = [k.sb([128, NT, 132], BF16, f"Vaug{i}") for i in range(2)]
        QKn2 = [k.sb([128, 2, S], BF16, f"QKn{i}") for i in range(2)]
        QKr2 = [k.sb([64, 2, S], BF16, f"QKr{i}") for i in range(2)]
        PT = [k.sb([128, 512], BF16, f"PT{i}") for i in range(2)]
        ob = k.sb([128, NT, 128], BF16, "ob")
        rsum = k.sb([128, 4], F32, "rsum")
        mixa = k.sb([128, S], BF16, "mixa")
        for i in range(2):
            k.op("pool", "memset", Vaug2[i][:, :, 128:132], 1.0, writes=[Vaug2[i]])

        def load_wqkv(h):
            k.dma("pool", out=wq[h % 2][:], in_=w_uq_d[:, h * 192:(h + 1) * 192].rearrange("(kc p) n -> p kc n", p=128),
                  writes=[wq[h % 2]])
            k.dma("pool", out=wkv[h % 2][:], in_=w_ukv_d[:, h * 256:(h + 1) * 256].rearrange("(kc p) n -> p kc n", p=128),
                  writes=[wkv[h % 2]])

        def prep_gen(h):
            wq_, wkv_ = wq[h % 2], wkv[h % 2]
            Vaug, QKn, QKr = Vaug2[h % 2], QKn2[h % 2], QKr2[h % 2]
            pb = PS[2]
            for t in range(NT):
                for kc in range(4):
                    k.op("pe", "matmul", pb[:, 0:192], lhsT=cqnT[:, kc, t * 128:(t + 1) * 128], rhs=wq_[:, kc, :],
                         start=(kc == 0), stop=(kc == 3), reads=[cqnT, wq_], writes=[pb], sig=False)
                for kc in range(2):
                    k.op("pe", "matmul", pb[:, 192:448], lhsT=ckvnT[:, kc, t * 128:(t + 1) * 128], rhs=wkv_[:, kc, :],
                         start=(kc == 0), stop=(kc == 1), reads=[ckvnT, wkv_], writes=[pb], sig=(kc == 1))
                k.op("act", "copy", raw[:, t, :], pb[:, 0:448], reads=[pb], writes=[raw])
                yield
            k.op("dve", "tensor_tensor", jk2[:], raw[:, :, 0:320], raw[:, :, 0:320], ALU.mult, reads=[raw], writes=[jk2])
            k.op("dve", "reduce_sum", ssq[:, 0, :], jk2[:, :, 0:192], AX.X, reads=[jk2], writes=[ssq])
            k.op("dve", "reduce_sum", ssq[:, 1, :], jk2[:, :, 192:320], AX.X, reads=[jk2], writes=[ssq])
            yield
            rstd_from_ss(ssq[:, 0, :], ssq[:, 4, :], ssq[:, 2, :], 1.0, SCQ * EPS, [ssq], ssq, ssq)
            k.op("dve", "tensor_tensor", ssq[:, 1, :], ssq[:, 1, :], sskpe[:], ALU.add, reads=[ssq, sskpe], writes=[ssq])
            rstd_from_ss(ssq[:, 1, :], ssq[:, 5, :], ssq[:, 3, :], 1.0 / SCQ, EPS, [ssq], ssq, ssq)
            rq_b = ssq[:, 4, :].unsqueeze(2)
            rk_b = ssq[:, 5, :].unsqueeze(2)
            yield
            k.op("dve", "tensor_tensor", raw[:, :, 0:192], raw[:, :, 0:192], rq_b.to_broadcast([128, NT, 192]), ALU.mult,
                 reads=[raw, ssq], writes=[raw])
            yield
            k.op("dve", "tensor_tensor", raw[:, :, 0:192], raw[:, :, 0:192],
                 qhg[:].unsqueeze(1).to_broadcast([128, NT, 192]), ALU.mult, reads=[raw, qhg], writes=[raw])
            yield
            k.op("dve", "tensor_copy", qb[:, :, 0:128], raw[:, :, 0:128], reads=[raw], writes=[qb])
            rope(qb[:, :, 128:160], qb[:, :, 160:192], raw[:, :, 128:160], raw[:, :, 160:192], cosb, sinb, rt1[:, 0],
                 rt2[:, 0], [raw], qb, None)
            yield
            k.op("dve", "tensor_tensor", raw[:, :, 192:320], raw[:, :, 192:320], rk_b.to_broadcast([128, NT, 128]), ALU.mult,
                 reads=[raw, ssq], writes=[raw])
            yield
            k.op("dve", "tensor_tensor", kb[:, :, 0:128], raw[:, :, 192:320],
                 khg[:, 0:128].unsqueeze(1).to_broadcast([128, NT, 128]), ALU.mult, reads=[raw, khg], writes=[kb])
            k.op("dve", "tensor_tensor", kb[:, :, 128:192], kper[:], rk_b.to_broadcast([128, NT, 64]), ALU.mult,
                 reads=[kper, ssq], writes=[kb])
            k.op("act", "copy", Vaug[:, :, 0:128], raw[:, :, 320:448], reads=[raw], writes=[Vaug])
            yield
            bank = 3
            for t in range(NT):
                k.op("pe", "transpose", PSB[bank][:, 0:128], qb[:, t, 0:128], identb[:], reads=[qb, identb], writes=[PS[bank]], sig=False)
                k.op("pe", "transpose", PSB[bank][:, 128:256], kb[:, t, 0:128], identb[:], reads=[kb, identb], writes=[PS[bank]], sig=False)
                k.op("pe", "transpose", PSB[bank][0:64, 256:384], qb[:, t, 128:192], identb[:], reads=[qb, identb], writes=[PS[bank]], sig=False)
                k.op("pe", "transpose", PSB[bank][0:64, 384:512], kb[:, t, 128:192], identb[:], reads=[kb, identb], writes=[PS[bank]])
                k.op("act", "copy", QKn[:, :, t * 128:(t + 1) * 128], PSB[bank][:, 0:256].rearrange("p (a n) -> p a n", a=2),
                     reads=[PS[bank]], writes=[QKn])
                k.op("act", "copy", QKr[:, :, t * 128:(t + 1) * 128], PSB[bank][0:64, 256:512].rearrange("p (a n) -> p a n", a=2),
                     reads=[PS[bank]], writes=[QKr])
                yield
            for wi in range(20):
                k.op("pe", "matmul", PS[2][:, :], lhsT=cqnT[:, 0, 0:128], rhs=cqnT[:, 0, 0:512], start=True, stop=True,
                     reads=[cqnT], writes=[PS[2]], sig=(wi == 19))
            yield

        def attn_gen(h):
            Vaug, QKn, QKr = Vaug2[h % 2], QKn2[h % 2], QKr2[h % 2]
            its = [(qblk, kt) for qblk in range(4) for kt in range(NT)]

            def scores(i):
                qblk, kt = its[i]
                qs = slice(qblk * 512, (qblk + 1) * 512)
                ks = slice(kt * 128, (kt + 1) * 128)
                sb_ = PS[i % 2]
                k.op("pe", "matmul", sb_[:, :], lhsT=QKn[:, 1, ks], rhs=QKn[:, 0, qs], start=True, stop=False,
                     reads=[QKn], writes=[sb_], sig=False)
                k.op("pe", "matmul", sb_[:, :], lhsT=QKr[:, 1, ks], rhs=QKr[:, 0, qs], start=False, stop=True,
                     reads=[QKr], writes=[sb_])
                k.op("act", "activation", PT[i % 2][:], sb_[:, :], AF.Exp, reads=[sb_], writes=[PT[i % 2]])

            scores(0)
            scores(1)
            for i, (qblk, kt) in enumerate(its):
                pt = PT[i % 2]
                for j in range(4):
                    k.op("pe", "matmul", PS[4 + j][:, 0:129], lhsT=pt[:, j * 128:(j + 1) * 128], rhs=Vaug[:, kt, 0:129],
                         start=(kt == 0), stop=(kt == NT - 1), reads=[pt, Vaug], writes=[PS[4 + j]],
                         sig=(j == 3))
                if i + 2 < len(its):
                    scores(i + 2)
                if kt == NT - 1:
                    for j in range(4):
                        qt_ = qblk * 4 + j
                        k.op("dve", "reciprocal", rsum[:, j:j + 1], PS[4 + j][:, 128:129], reads=[PS[4 + j]], writes=[rsum])
                        k.op("dve", "tensor_scalar", ob[:, qt_, :], PS[4 + j][:, 0:128], rsum[:, j:j + 1], None, ALU.mult,
                             reads=[PS[4 + j], rsum], writes=[ob])
                yield
            for half in range(2):
                transpose_evac(mixa[:, half * 1024:(half + 1) * 1024], mixa, [ob[:, half * 8 + i, :] for i in range(8)], ob,
                               3)
                yield
            k.dma("sp", out=mixT_d[:, :, 8 + h, :].rearrange("t p n -> p t n"), in_=mixa[:].rearrange("p (t n) -> p t n", n=128),
                  reads=[mixa], writes=[R_mixT])

        load_wqkv(0)
        for _ in prep_gen(0):
            pass
        for h in range(8):
            pn = None
            if h + 1 < 8:
                load_wqkv(h + 1)
                pn = prep_gen(h + 1)
            for _ in attn_gen(h):
                if pn is not None:
                    next(pn, None)
            if pn is not None:
                for _ in pn:
                    pass
        k.barrier()
        k.release(m4)
        if stop_after <= 4:
            return nc, k

        m5 = k.mark()
        wo = k.sb([128, 16, D], BF16, "wo")
        for nbk in range(4):
            k.dma("pool", out=wo[:, :, nbk * 512:(nbk + 1) * 512],
                  in_=w_out_d[:, nbk * 512:(nbk + 1) * 512].rearrange("(kc p) n -> p kc n", p=128), writes=[wo])
        wr = k.sb([128, 16, 16], BF16, "wr")
        k.dma("pool", out=wr[:], in_=w_rt_d.rearrange("(kc p) n -> p kc n", p=128), writes=[wr])
        g1b = k.sb([128, D], F32, "g1b")
        G2b = k.sb([128, D], F32, "G2b")
        Sh2b = k.sb([128, D], F32, "Sh2b")
        bcast_load(g1b, 2 * D)
        bcast_load(G2b, 4 * D)
        bcast_load(Sh2b, 3 * D)
        affT = k.sb([16, S], F32, "affT")
        mx = [k.sb([128, 16, 128], BF16, f"mx{i}") for i in range(2)]
        xt5 = [k.sb([128, D], F32, f"xt5{i}") for i in range(2)]
        x1 = [k.sb([128, D], F32, f"x1{i}") for i in range(2)]
        tm5 = k.sb([128, D], F32, "tm5")
        jk5 = k.sb([128, D], BF16, "jk5")
        h2b = [k.sb([128, D], BF16, f"h2b{i}") for i in range(2)]
        h2T = k.sb([128, 16, 128], BF16, "h2T")
        st5 = [k.sb([128, 8], F32, f"st5{i}") for i in range(2)]
        lg = [k.sb([128, 16], F32, f"lg{i}") for i in range(2)]
        def s5_mm(t):
            ts_ = slice(t * 128, (t + 1) * 128)
            m_, xx = mx[t % 2], xt5[t % 2]
            k.dma("sp", out=m_[:], in_=mixT_d[t], reads=[R_mixT], writes=[m_])
            k.dma("sp", out=xx[:], in_=x_d[ts_, :], writes=[xx])
            for nbk in range(4):
                pb = PS[nbk]
                cs_ = slice(nbk * 512, (nbk + 1) * 512)
                for kc in range(16):
                    k.op("pe", "matmul", pb[:, :], lhsT=m_[:, kc, :], rhs=wo[:, kc, cs_], start=(kc == 0), stop=(kc == 15),
                         reads=[m_, wo], writes=[pb], sig=(kc == 15))

        def s5_evac(t):
            ts_ = slice(t * 128, (t + 1) * 128)
            xx, x1_ = xt5[t % 2], x1[t % 2]
            for nbk in range(4):
                pb = PS[nbk]
                cs_ = slice(nbk * 512, (nbk + 1) * 512)
                k.op("dve", "tensor_tensor", tm5[:, cs_], pb[:, :], g1b[:, cs_], ALU.mult, reads=[pb, g1b], writes=[tm5])
                k.op("dve", "tensor_tensor", x1_[:, cs_], tm5[:, cs_], xx[:, cs_], ALU.add, reads=[tm5, xx], writes=[x1_])
            k.dma("pool", out=out_d[ts_, :], in_=x1_[:], reads=[x1_], writes=[R_out])

        def s5_norm(t):
            ts_ = slice(t * 128, (t + 1) * 128)
            x1_, hb_, s_ = x1[t % 2], h2b[t % 2], st5[t % 2]
            k.op("act", "activation", jk5[:], x1_[:], AF.Square, accum_out=s_[:, 0:1], reads=[x1_], writes=[jk5, s_])
            rstd_from_ss(s_[:, 0:1], s_[:, 2:3], s_[:, 1:2], 1.0 / D, EPS, [s_], s_, s_)
            k.op("dve", "scalar_tensor_tensor", tm6[:], x1_[:], s_[:, 2:3], G2b[:], ALU.mult, ALU.mult,
                 reads=[x1_, s_, G2b], writes=[tm6])
            k.op("dve", "tensor_tensor", hb_[:], tm6[:], Sh2b[:], ALU.add, reads=[tm6, Sh2b], writes=[hb_])
            k.dma("pool", out=h2_d[ts_, :], in_=hb_[:], reads=[hb_], writes=[R_h2])

        def s5_router(t):
            ts_ = slice(t * 128, (t + 1) * 128)
            hb_, s_, lg_ = h2b[t % 2], st5[t % 2], lg[t % 2]
            for half in range(2):
                transpose_evac(h2T[:, half * 8:(half + 1) * 8, :], h2T, [hb_[:, (half * 8 + i) * 128:(half * 8 + i + 1) * 128]
                                                                       for i in range(8)], hb_, 4 + half)
            pl = PS[6]
            for kc in range(16):
                k.op("pe", "matmul", pl[:, 0:16], lhsT=h2T[:, kc, :], rhs=wr[:, kc, :], start=(kc == 0), stop=(kc == 15),
                     reads=[h2T, wr], writes=[pl], sig=(kc == 15))
            k.op("dve", "reduce_max", s_[:, 3:4], pl[:, 0:16], AX.X, reads=[pl], writes=[s_])
            k.op("dve", "tensor_scalar", s_[:, 4:5], s_[:, 3:4], -1.0, None, ALU.mult, reads=[s_], writes=[s_])
            k.op("act", "activation", lg_[:], pl[:, 0:16], AF.Exp, bias=s_[:, 4:5], accum_out=s_[:, 5:6], reads=[pl, s_],
                 writes=[lg_, s_])
            k.op("dve", "reciprocal", s_[:, 6:7], s_[:, 5:6], reads=[s_], writes=[s_])
            k.op("dve", "tensor_scalar", lg_[:], lg_[:], s_[:, 6:7], None, ALU.mult, reads=[lg_, s_], writes=[lg_])
            pt_ = PS[7]
            k.op("pe", "transpose", pt_[0:16, 0:128], lg_[:], identf[:], reads=[lg_, identf], writes=[pt_])
            k.op("act", "copy", affT[:, ts_], pt_[0:16, 0:128], reads=[pt_], writes=[affT])

        tm6 = k.sb([128, D], F32, "tm6")
        s5_mm(0)
        s5_evac(0)
        for t in range(NT):
            if t + 1 < NT:
                s5_mm(t + 1)
            s5_norm(t)
            if t + 1 < NT:
                s5_evac(t + 1)
            s5_router(t)
        if debug:
            k.dma("sp", out=aff_d, in_=affT[:], reads=[affT], writes=[R_aff])
        k.barrier()
        if stop_after <= 5:
            return nc, k

        CAP = 256
        wk_ = k.sb([16, S], F32, "wk")
        vals = k.sb([16, CAP], F32, "vals")
        idxu = k.sb([16, CAP], U32, "idxu")
        idxf = k.sb([16, CAP], F32, "idxf")
        k.op("dve", "tensor_copy", wk_[:], affT[:], reads=[affT], writes=[wk_])
        for r in range(CAP // 8):
            sl = slice(r * 8, (r + 1) * 8)
            k.op("dve", "max", vals[:, sl], wk_[:], reads=[wk_], writes=[vals])
            k.op("dve", "max_index", idxu[:, sl], vals[:, sl], wk_[:], reads=[wk_, vals], writes=[idxu])
            if r < CAP // 8 - 1:
                k.op("dve", "match_replace", wk_[:], vals[:, sl], wk_[:], -1.0, reads=[wk_, vals], writes=[wk_])
        k.op("dve", "tensor_copy", idxf[:], idxu[:], reads=[idxu], writes=[idxf])
        idxT = k.sb([128, 2, 16], I32, "idxT")
        gateT = k.sb([128, 2, 16], F32, "gateT")
        for hf in range(2):
            pa = PS[hf]
            k.op("pe", "transpose", pa[:, 0:16], idxf[:, hf * 128:(hf + 1) * 128], identf[0:16, 0:16], reads=[idxf, identf],
                 writes=[pa], sig=False)
            k.op("pe", "transpose", pa[:, 16:32], vals[:, hf * 128:(hf + 1) * 128], identf[0:16, 0:16], reads=[vals, identf],
                 writes=[pa])
            k.op("dve", "tensor_copy", idxT[:, hf, :], pa[:, 0:16], reads=[pa], writes=[idxT])
            k.op("dve", "tensor_copy", gateT[:, hf, :], pa[:, 16:32], reads=[pa], writes=[gateT])
        k.barrier()
        k.release(m5)
        idxT2 = k.sb([128, 2, 16], I32, "idxT2")
        gateT2 = k.sb([128, 2, 16], F32, "gateT2")
        k.op("dve", "tensor_copy", idxT2[:], idxT[:], reads=[idxT], writes=[idxT2])
        k.op("dve", "tensor_copy", gateT2[:], gateT[:], reads=[gateT], writes=[gateT2])
        k.barrier()
        if stop_after <= 6:
            return nc, k

        g2b = k.sb([128, D], F32, "g2b")
        bcast_load(g2b, 5 * D)
        xe = [k.sb([128, D], BF16, f"xe{i}") for i in range(2)]
        xeT = k.sb([128, 16, CAP], BF16, "xeT")
        wg = [k.sb([128, 16, 512], BF16, f"wg{i}") for i in range(2)]
        wu = [k.sb([128, 16, 512], BF16, f"wu{i}") for i in range(2)]
        wd = [k.sb([128, 16, 512], BF16, f"wd{i}") for i in range(2)]
        hTe = k.sb([128, 16, CAP], BF16, "hTe")
        sa = [k.sb([128, CAP], F32, f"sa{i}") for i in range(2)]
        yst = [k.sb([128, D], F32, f"yst{i}") for i in range(2)]
        gu_cnt = [0]
        gu_buf = {}
        wd_buf = {}

        def gather(e):
            for hf in range(2):
                k.dma("pool", meth="indirect_dma_start", out=xe[hf][:], out_offset=None, in_=h2_d,
                      in_offset=bass.IndirectOffsetOnAxis(ap=idxT2[:, hf, e:e + 1], axis=0), reads=[idxT2, R_h2],
                      writes=[xe[hf]])

        def load_gu(e, fb):
            wg_, wu_ = wg[gu_cnt[0] % 2], wu[gu_cnt[0] % 2]
            gu_cnt[0] += 1
            fsl = slice(fb * 512, (fb + 1) * 512)
            k.dma("pool", out=wg_[:], in_=w_gate_d[e, :, fsl].rearrange("(kc p) n -> p kc n", p=128), writes=[wg_])
            k.dma("pool", out=wu_[:], in_=w_up_d[e, :, fsl].rearrange("(kc p) n -> p kc n", p=128), writes=[wu_])
            gu_buf[(e, fb)] = (wg_, wu_)

        def load_wd(e, db):
            wd_ = wd[db % 2]
            dsl = slice(db * 512, (db + 1) * 512)
            k.dma("pool", out=wd_[:], in_=w_down_d[e, :, dsl].rearrange("(fc p) n -> p fc n", p=128), writes=[wd_])
            wd_buf[(e, db)] = wd_

        gather(0)
        load_gu(0, 0)
        for e in range(16):
            for hf in range(2):
                for half in range(2):
                    transpose_evac(xeT[:, half * 8:(half + 1) * 8, hf * 128:(hf + 1) * 128], xeT,
                                   [xe[hf][:, (half * 8 + i) * 128:(half * 8 + i + 1) * 128] for i in range(8)], xe[hf],
                                   6 + half)
            for fb in range(4):
                if fb + 1 < 4:
                    load_gu(e, fb + 1)
                else:
                    load_wd(e, 0)
                wg_, wu_ = gu_buf.pop((e, fb))
                for fs in range(4):
                    pa, pu = PS[(fs % 2) * 2], PS[(fs % 2) * 2 + 1]
                    for kc in range(16):
                        k.op("pe", "matmul", pa[:, 0:CAP], lhsT=wg_[:, kc, fs * 128:(fs + 1) * 128], rhs=xeT[:, kc, :],
                             start=(kc == 0), stop=(kc == 15), reads=[wg_, xeT], writes=[pa], sig=(kc == 15))
                    for kc in range(16):
                        k.op("pe", "matmul", pu[:, 0:CAP], lhsT=wu_[:, kc, fs * 128:(fs + 1) * 128], rhs=xeT[:, kc, :],
                             start=(kc == 0), stop=(kc == 15), reads=[wu_, xeT], writes=[pu], sig=(kc == 15))
                    s_ = sa[fs % 2]
                    k.op("act", "activation", s_[:], pa[:, 0:CAP], AF.Silu, reads=[pa], writes=[s_])
                    k.op("dve", "tensor_tensor", hTe[:, fb * 4 + fs, :], s_[:], pu[:, 0:CAP], ALU.mult, reads=[s_, pu],
                         writes=[hTe])
            for db in range(4):
                if db + 1 < 4:
                    load_wd(e, db + 1)
                elif e + 1 < 16:
                    gather(e + 1)
                    load_gu(e + 1, 0)
                wd_ = wd_buf.pop((e, db))
                dsl = slice(db * 512, (db + 1) * 512)
                for hf in range(2):
                    py = PS[4 + hf]
                    for fc in range(16):
                        k.op("pe", "matmul", py[:, :], lhsT=hTe[:, fc, hf * 128:(hf + 1) * 128], rhs=wd_[:, fc, :],
                             start=(fc == 0), stop=(fc == 15), reads=[hTe, wd_], writes=[py], sig=(fc == 15))
                    k.op("dve", "scalar_tensor_tensor", yst[hf][:, dsl], py[:, :], gateT2[:, hf, e:e + 1], g2b[:, dsl],
                         ALU.mult, ALU.mult, reads=[py, gateT2, g2b], writes=[yst[hf]])
            for hf in range(2):
                k.dma("pool", meth="indirect_dma_start", out=out_d, out_offset=bass.IndirectOffsetOnAxis(
                    ap=idxT2[:, hf, e:e + 1], axis=0), in_=yst[hf][:], in_offset=None, compute_op=ALU.add,
                    reads=[idxT2, yst[hf], R_out], writes=[R_out])
        k.barrier()
        return nc, k


_CACHE = {}


def _prep(inputs, b):
    f = lambda a: np.ascontiguousarray(a, dtype=np.float32)
    d = {}
    d["x"] = f(inputs["x"][b])
    d["c_t"] = f(inputs["c"][b].reshape(16, 128).T)
    d["pos_t"] = np.ascontiguousarray(inputs["positions"][b].reshape(16, 128).T.astype(np.int32))
    d["w_ada"] = f(inputs["w_ada"][0])
    d["b_ada"] = f(inputs["b_ada"][0].reshape(1, -1))
    d["norm1_g"] = f(inputs["norm1_g"][0].reshape(1, -1))
    d["w_in"] = f(inputs["w_in"][0])
    d["lbl"] = f(inputs["lb_logits"].reshape(2, 2, 8, 128).transpose(3, 0, 1, 2).reshape(128, 32))
    d["hog"] = f(inputs["hgrn_out_g"][0].T)
    d["qa_g"] = f(inputs["qa_norm_g"][0].reshape(1, -1))
    d["w_uq"] = f(inputs["w_uq"][0])
    d["kva_g"] = f(inputs["kva_norm_g"][0].reshape(1, -1))
    d["w_ukv"] = f(inputs["w_ukv"][0])
    d["q_hg"] = f(inputs["q_head_g"][0].reshape(1, -1))
    d["k_hg"] = f(inputs["k_head_g"][0].reshape(1, -1))
    d["w_out"] = f(inputs["w_out"][0])
    d["norm2_g"] = f(inputs["norm2_g"][0].reshape(1, -1))
    d["w_router"] = f(inputs["w_router"][0])
    d["w_gate"] = f(inputs["w_gate"][0])
    d["w_up"] = f(inputs["w_up"][0])
    d["w_down"] = f(inputs["w_down"][0])
    invf = 1.0 / (10000.0 ** (np.arange(0, 64, 2, dtype=np.float32) / 64.0))
    d["inv_freq"] = (invf / np.float32(2 * np.pi)).astype(np.float32).reshape(1, 32)
    return d


def kernel(**inputs):
    inputs = {k_: np.asarray(v) for k_, v in inputs.items()}
    if "nc" not in _CACHE:
        nc, kk = build()
        kk.replay()
        _CACHE["nc"] = nc
    nc = _CACHE["nc"]
    shared = None
    in_maps = []
    for core in range(8):
        b = core % 4
        if core < 4:
            in_maps.append(_prep(inputs, b))
        else:
            in_maps.append(in_maps[b])
    res = run_bass_kernel_spmd(nc, in_maps, core_ids=list(range(8)))
    out = np.stack([np.asarray(res.results[b]["out"], dtype=np.float32) for b in range(4)], axis=0)
    return out
```
